# Optimizing a Trainium2 kernel written in Bass

```python
import math
import jax, jax.numpy as jnp
from jax import lax
import numpy as np

D_MODEL = 4096
BATCH = 2
SEQ = 8192
DEPTH = 2

N_A_LAYERS = DEPTH // 2
N_B_LAYERS = DEPTH - N_A_LAYERS

RWKV_HEAD_DIM = 64
RWKV_HEADS = D_MODEL // RWKV_HEAD_DIM
DECAY_LORA = max(32, int(round(1.8 * D_MODEL ** 0.5 / 32)) * 32)
ICLR_LORA = max(32, int(round(1.8 * D_MODEL ** 0.5 / 32)) * 32)
N_SHIFT_MIX = 6
GN_EPS = 64e-5

MOBA_HEAD_DIM = 128
MOBA_HEADS = D_MODEL // MOBA_HEAD_DIM
MOBA_BLOCK = 256
MOBA_TOPK = 3
Q_CHUNK = 32

NORM_EPS = 1e-6

kernel_name = "yoco_rwkv7_moba_hybrid"


def _rmsnorm(x, g):
    xf = x.astype(jnp.float32)
    y = xf * lax.rsqrt(jnp.mean(xf * xf, axis=-1, keepdims=True) + NORM_EPS)
    return (y * g.astype(jnp.float32)).astype(x.dtype)


def _rwkv7_scan(r, decay, k, v, kk, kka):
    B, T, H, N = r.shape

    def step(S, inp):
        r_t, w_t, k_t, v_t, kk_t, kka_t = inp
        sa = jnp.einsum('bhvk,bhk->bhv', S, -kk_t)
        S = (S * w_t[:, :, None, :] + sa[..., None] * kka_t[:, :, None, :]
             + v_t[..., None] * k_t[:, :, None, :])
        y_t = jnp.einsum('bhvk,bhk->bhv', S, r_t)
        return S, y_t

    xs = (jnp.moveaxis(r, 1, 0), jnp.moveaxis(decay, 1, 0), jnp.moveaxis(k, 1, 0),
          jnp.moveaxis(v, 1, 0), jnp.moveaxis(kk, 1, 0), jnp.moveaxis(kka, 1, 0))
    S0 = jnp.zeros((B, H, N, N), jnp.float32)
    _, y = lax.scan(step, S0, xs)
    return jnp.moveaxis(y, 0, 1)


def _rwkv7_mixer(hn, mu, w_in, w0, w1, w2, a0, a1, a2, k_k, k_a, r_k, lnx_w, lnx_b, w_o):
    f32 = jnp.float32
    B, T, D = hn.shape
    H, N = RWKV_HEADS, RWKV_HEAD_DIM
    dx = jnp.pad(hn, ((0, 0), (1, 0), (0, 0)))[:, :T] - hn

    def lerp(i):
        return hn + dx * mu[i]

    r = lerp(0) @ w_in[0]
    k = lerp(1) @ w_in[1]
    v = lerp(2) @ w_in[2]
    gate = lerp(3) @ w_in[3]
    wlog = -jax.nn.softplus(-(w0 + jnp.tanh(lerp(4) @ w1) @ w2).astype(f32)) - 0.5
    decay = jnp.exp(-jnp.exp(wlog))
    a = jax.nn.sigmoid((a0 + (lerp(5) @ a1) @ a2).astype(f32))

    r = r.astype(f32).reshape(B, T, H, N)
    k = k.astype(f32).reshape(B, T, H, N)
    v = v.astype(f32).reshape(B, T, H, N)
    a = a.reshape(B, T, H, N)
    decay = decay.reshape(B, T, H, N)

    kk = k * k_k.astype(f32).reshape(H, N)
    kk = kk / jnp.maximum(jnp.sqrt(jnp.sum(kk * kk, axis=-1, keepdims=True)), 1e-12)
    k = k * (1.0 + (a - 1.0) * k_a.astype(f32).reshape(H, N))

    y = _rwkv7_scan(r, decay, k, v, kk, kk * a)

    mean = jnp.mean(y, axis=-1, keepdims=True)
    var = jnp.mean(jnp.square(y - mean), axis=-1, keepdims=True)
    y = ((y - mean) * lax.rsqrt(var + GN_EPS)).reshape(B, T, D) * lnx_w.astype(f32) + lnx_b.astype(f32)
    bonus = jnp.sum(r * k * r_k.astype(f32), axis=-1, keepdims=True) * v
    y = (y + bonus.reshape(B, T, D)) * jax.nn.silu(gate.astype(f32))
    return y.astype(hn.dtype) @ w_o


def _shared_kv(h, kv_norm_g, w_k, w_v):
    B, T, D = h.shape
    H, Dh = MOBA_HEADS, MOBA_HEAD_DIM
    nb = -(-T // MOBA_BLOCK)
    pad = nb * MOBA_BLOCK - T
    hn = _rmsnorm(h, kv_norm_g)

    def blocks(t):
        t = jnp.pad(t, ((0, 0), (0, pad), (0, 0)))
        return t.reshape(B, nb, MOBA_BLOCK, H, Dh).transpose(0, 3, 1, 2, 4)

    kb = blocks(hn @ w_k)
    vb = blocks(hn @ w_v)
    kmean = jnp.mean(kb.astype(jnp.float32), axis=3).astype(kb.dtype)
    return kb, vb, kmean


def _moba_attend(q, kb, vb, kmean):
    f32 = jnp.float32
    B, H, T, Dh = q.shape
    nb = kb.shape[2]
    n_sel = min(MOBA_TOPK, nb)
    scale = Dh ** -0.5
    slopes = jnp.exp2(-8.0 * jnp.arange(1, H + 1, dtype=f32) / H)
    b_idx = jnp.arange(B)[:, None, None, None]
    h_idx = jnp.arange(H)[None, :, None, None]
    offs = jnp.arange(MOBA_BLOCK)
    blk_ids = jnp.arange(nb)

    def chunk(c):
        t0 = c * Q_CHUNK
        qc = lax.dynamic_slice_in_dim(q, t0, Q_CHUNK, axis=2)
        tpos = t0 + jnp.arange(Q_CHUNK)
        own = t0 // MOBA_BLOCK
        gate = jnp.einsum('bhcd,bhnd->bhcn', qc, kmean).astype(f32)
        gate = jnp.where(blk_ids < own, gate, -jnp.inf)
        gval, sel = lax.top_k(gate, n_sel)
        valid = gval > -jnp.inf
        k_sel = kb[b_idx, h_idx, sel]
        v_sel = vb[b_idx, h_idx, sel]
        kpos = sel[..., None] * MOBA_BLOCK + offs
        dist = (tpos[None, None, :, None, None] - kpos).astype(f32)
        s_sel = (jnp.einsum('bhcd,bhcjsd->bhcjs', qc, k_sel).astype(f32) * scale
                 - slopes[None, :, None, None, None] * dist)
        s_sel = jnp.where(valid[..., None], s_sel, -jnp.inf).reshape(B, H, Q_CHUNK, n_sel * MOBA_BLOCK)
        k_own = lax.dynamic_index_in_dim(kb, own, axis=2, keepdims=False)
        v_own = lax.dynamic_index_in_dim(vb, own, axis=2, keepdims=False)
        kpos_own = own * MOBA_BLOCK + offs
        dist_own = (tpos[:, None] - kpos_own[None, :]).astype(f32)
        s_own = (jnp.einsum('bhcd,bhsd->bhcs', qc, k_own).astype(f32) * scale
                 - slopes[None, :, None, None] * dist_own[None, None])
        s_own = jnp.where((dist_own >= 0)[None, None], s_own, -jnp.inf)
        p = jax.nn.softmax(jnp.concatenate([s_sel, s_own], axis=-1), axis=-1)
        p_sel = p[..., :n_sel * MOBA_BLOCK].reshape(B, H, Q_CHUNK, n_sel, MOBA_BLOCK).astype(vb.dtype)
        p_own = p[..., n_sel * MOBA_BLOCK:].astype(vb.dtype)
        return (jnp.einsum('bhcjs,bhcjsd->bhcd', p_sel, v_sel)
                + jnp.einsum('bhcs,bhsd->bhcd', p_own, v_own))

    out = lax.map(chunk, jnp.arange(T // Q_CHUNK))
    return out.transpose(1, 0, 3, 2, 4).reshape(B, T, H * Dh)


def _moba_mixer(hn, w_qg, w_o, kb, vb, kmean):
    B, T, D = hn.shape
    qg = hn @ w_qg
    q = qg[..., :D].reshape(B, T, MOBA_HEADS, MOBA_HEAD_DIM).transpose(0, 2, 1, 3)
    gate = qg[..., D:]
    att = _moba_attend(q, kb, vb, kmean)
    y = att.astype(jnp.float32) * jax.nn.silu(gate.astype(jnp.float32))
    return y.astype(hn.dtype) @ w_o


def setup_inputs(seed: int = 0) -> dict:
    key = jax.random.key(seed)
    ks = jax.random.split(key, 24)
    f32 = jnp.float32
    D, NA, NBL = D_MODEL, N_A_LAYERS, N_B_LAYERS
    s = D ** -0.5

    def nrm(k, shape, scale):
        return scale * jax.random.normal(k, shape, f32)

    def gain(k, shape):
        return 1.0 + 0.02 * jax.random.normal(k, shape, f32)

    return {
        "x": jax.random.normal(ks[0], (BATCH, SEQ, D), f32),
        "a_pre_g": gain(ks[1], (NA, D)),
        "a_post_g": gain(ks[2], (NA, D)),
        "a_mu": jax.random.uniform(ks[3], (NA, N_SHIFT_MIX, D), f32),
        "a_w_in": nrm(ks[4], (NA, 4, D, D), s),
        "a_w0": jax.random.uniform(ks[5], (NA, D), f32, minval=-6.0, maxval=1.0),
        "a_w1": nrm(ks[6], (NA, D, DECAY_LORA), s),
        "a_w2": nrm(ks[7], (NA, DECAY_LORA, D), 0.5 * DECAY_LORA ** -0.5),
        "a_a0": nrm(ks[8], (NA, D), 0.1),
        "a_a1": nrm(ks[9], (NA, D, ICLR_LORA), s),
        "a_a2": nrm(ks[10], (NA, ICLR_LORA, D), 0.5 * ICLR_LORA ** -0.5),
        "a_k_k": 0.85 + nrm(ks[11], (NA, D), 0.05),
        "a_k_a": 1.0 + nrm(ks[12], (NA, D), 0.05),
        "a_r_k": nrm(ks[13], (NA, RWKV_HEADS, RWKV_HEAD_DIM), 0.1),
        "a_lnx_w": gain(ks[14], (NA, D)),
        "a_lnx_b": nrm(ks[15], (NA, D), 0.02),
        "a_w_o": nrm(ks[16], (NA, D, D), s),
        "kv_norm_g": gain(ks[17], (D,)),
        "w_k": nrm(ks[18], (D, D), s),
        "w_v": nrm(ks[19], (D, D), s),
        "b_pre_g": gain(ks[20], (NBL, D)),
        "b_post_g": gain(ks[21], (NBL, D)),
        "b_w_qg": nrm(ks[22], (NBL, D, 2 * D), s),
        "b_w_o": nrm(ks[23], (NBL, D, D), s),
    }


def reference(x, a_pre_g, a_post_g, a_mu, a_w_in, a_w0, a_w1, a_w2, a_a0, a_a1, a_a2,
              a_k_k, a_k_a, a_r_k, a_lnx_w, a_lnx_b, a_w_o, kv_norm_g, w_k, w_v,
              b_pre_g, b_post_g, b_w_qg, b_w_o):
    h = x
    kb = vb = kmean = None
    for layer in range(DEPTH):
        if layer < N_A_LAYERS:
            i = layer
            mix = _rwkv7_mixer(_rmsnorm(h, a_pre_g[i]), a_mu[i], a_w_in[i], a_w0[i], a_w1[i],
                               a_w2[i], a_a0[i], a_a1[i], a_a2[i], a_k_k[i], a_k_a[i],
                               a_r_k[i], a_lnx_w[i], a_lnx_b[i], a_w_o[i])
            h = h + _rmsnorm(mix, a_post_g[i])
        else:
            j = layer - N_A_LAYERS
            if j == 0:
                kb, vb, kmean = _shared_kv(h, kv_norm_g, w_k, w_v)
            mix = _moba_mixer(_rmsnorm(h, b_pre_g[j]), b_w_qg[j], b_w_o[j], kb, vb, kmean)
            h = h + _rmsnorm(mix, b_post_g[j])
    return h
```

```python
import contextlib, math
import numpy as np
import concourse.bass as bass
import concourse.mybir as mybir

F32 = mybir.dt.float32
BF16 = mybir.dt.bfloat16
ALU = mybir.AluOpType
AF = mybir.ActivationFunctionType
AX = mybir.AxisListType

SEM_ROT = 30000
N_DMA_SEMS = 40


class Trk:
    __slots__ = ("w", "r", "name")

    ALL = []

    def __init__(self, name=""):
        self.w = []
        self.r = []
        self.name = name
        Trk.ALL.append(self)


class Prog:
    def __init__(self, nc, stack):
        Trk.ALL.clear()
        self.nc = nc
        self.stack = stack
        self.E = {"pe": nc.tensor, "act": nc.scalar, "dve": nc.vector, "pool": nc.gpsimd, "sp": nc.sync}
        self.sems = {}
        self.cur = {}
        self.cnt = {}
        self.seen = {e: {} for e in self.E}
        self.nsem = 0
        for e in self.E:
            self._new_eng_sem(e)
        self.dma_keys = []
        for i in range(N_DMA_SEMS):
            k = f"dma{i}"
            self.sems[k] = stack.enter_context(nc.semaphore(k))
            self.cnt[k] = 0
            self.dma_keys.append(k)
        self.dma_rr = 0
        self.n_inst = 0

    def _new_eng_sem(self, e):
        k = f"s_{e}_{self.nsem}"
        self.nsem += 1
        self.sems[k] = self.stack.enter_context(self.nc.semaphore(k))
        self.cnt[k] = 0
        self.cur[e] = k

    def _wait(self, e, tok):
        k, v = tok
        if self.seen[e].get(k, 0) >= v:
            return
        self.seen[e][k] = v
        self.E[e].wait_ge(self.sems[k], v)

    def _deps(self, e, reads, writes, same_eng_raw=True):
        own = self.cur[e]
        for t in reads:
            for tok in t.w:
                if tok[0] == own and not same_eng_raw:
                    continue
                self._wait(e, tok)
        for t in writes:
            for tok in t.w:
                if tok[0] == own and not same_eng_raw:
                    continue
                self._wait(e, tok)
            for tok in t.r:
                if tok[0] == own:
                    continue
                self._wait(e, tok)

    def _commit(self, tok, reads, writes):
        for t in reads:
            t.r = [x for x in t.r if x[0] != tok[0]] + [tok]
        for t in writes:
            t.w = [tok]
            t.r = []

    def op(self, e, fn, reads=(), writes=(), same_eng_raw=True):
        if self.cnt[self.cur[e]] >= SEM_ROT:
            self._new_eng_sem(e)
        self._deps(e, reads, writes, same_eng_raw)
        k = self.cur[e]
        inst = fn(self.E[e])
        self.cnt[k] += 1
        inst.then_inc(self.sems[k], 1)
        tok = (k, self.cnt[k])
        self._commit(tok, reads, writes)
        self.n_inst += 1
        return tok

    def dma(self, q, out, in_, reads=(), writes=(), **kw):
        k = self.dma_keys[self.dma_rr % len(self.dma_keys)]
        self.dma_rr += 1
        if self.cnt[k] > 0:
            self._wait(q, (k, self.cnt[k]))
        for t in reads:
            for tok in t.w:
                self._wait(q, tok)
        for t in writes:
            for tok in t.w + t.r:
                self._wait(q, tok)
        inst = self.E[q].dma_start(out=out, in_=in_, **kw)
        self.cnt[k] += 16
        inst.then_inc(self.sems[k], 16)
        tok = (k, self.cnt[k])
        self._commit(tok, reads, writes)
        self.n_inst += 1
        return tok

    def wait_everything(self, e="sp"):
        self.wait_all(e, Trk.ALL)
        Trk.ALL.clear()

    def wait_all(self, e, trks):
        for t in trks:
            for tok in t.w + t.r:
                self._wait(e, tok)

    def sb(self, name, shape, dt):
        return self.stack.enter_context(self.nc.sbuf_tensor(name, list(shape), dt))

    def ps(self, name, shape, dt=F32):
        return self.stack.enter_context(self.nc.psum_tensor(name, list(shape), dt))


C_DEC = 0.6065306597126334
GN_EPS = 64e-5


def build_scan(NCH, out_dt=F32):
    nc = bass.Bass("TRN2", target_bir_lowering=False)
    fm = nc.dram_tensor("fm", [NCH, 128, 3, 8, 128], F32, kind="ExternalInput").ap()
    tm = nc.dram_tensor("tm", [NCH, 128, 3, 1024], F32, kind="ExternalInput").ap()
    cv = nc.dram_tensor("cv", [128, 3, 8], F32, kind="ExternalInput").ap()
    ln = nc.dram_tensor("ln", [2, 1024], F32, kind="ExternalInput").ap()
    yo = nc.dram_tensor("y", [NCH, 128, 1024], out_dt, kind="ExternalOutput").ap()
    with contextlib.ExitStack() as st:
        P = Prog(nc, st)
        sb, ps = P.sb, P.ps
        tC = Trk("const")
        ones_f = sb("ones_f", [128, 256], F32)
        negc = sb("negc", [128, 128], F32)
        ident_f = sb("ident_f", [128, 128], F32)
        ident_b = sb("ident_b", [128, 128], BF16)
        idpair = sb("idpair", [128, 64], F32)
        maskl = sb("maskl", [128, 128], F32)
        masku4 = sb("masku4", [128, 512], F32)
        tri2 = sb("tri2", [128, 256], F32)
        blk1 = sb("blk1", [128, 128], F32)
        onesblk = sb("onesblk", [128, 2], BF16)
        cvS = sb("cvS", [128, 3, 8], F32)
        omk = sb("omk", [128, 8], F32)
        lnS = sb("lnS", [128, 2, 1024], F32)
        G = P.E["pool"]
        P.op("pool", lambda e: e.memset(ones_f[:], 1.0), writes=[tC])
        P.op("pool", lambda e: e.memset(negc[:], -C_DEC), writes=[tC])
        P.op("pool", lambda e: e.affine_select(out=ident_f[:], in_=ones_f[:, 0:128], pattern=[[-1, 128]],
                                               compare_op=ALU.is_equal, fill=0.0, base=0, channel_multiplier=1), reads=[tC], writes=[tC])
        P.op("pool", lambda e: e.tensor_copy(out=ident_b[:], in_=ident_f[:]), reads=[tC], writes=[tC])
        P.op("pool", lambda e: e.tensor_copy(out=idpair[0:64, :], in_=ident_f[0:64, 0:64]), reads=[tC], writes=[tC])
        P.op("pool", lambda e: e.tensor_copy(out=idpair[64:128, :], in_=ident_f[64:128, 64:128]), reads=[tC], writes=[tC])
        P.op("pool", lambda e: e.affine_select(out=maskl[:], in_=ones_f[:, 0:128], pattern=[[-1, 128]],
                                               compare_op=ALU.is_gt, fill=0.0, base=0, channel_multiplier=1), reads=[tC], writes=[tC])
        for q in range(2):
            P.op("pool", lambda e: e.affine_select(out=masku4[:, q * 256:q * 256 + 128], in_=ones_f[:, 0:128], pattern=[[1, 128]],
                                                   compare_op=ALU.is_gt, fill=0.0, base=0, channel_multiplier=-1), reads=[tC], writes=[tC])
            P.op("pool", lambda e: e.affine_select(out=masku4[:, q * 256 + 128:q * 256 + 256], in_=ones_f[:, 0:128], pattern=[[1, 128]],
                                                   compare_op=ALU.is_ge, fill=0.0, base=0, channel_multiplier=-1), reads=[tC], writes=[tC])
        P.op("pool", lambda e: e.affine_select(out=tri2[:, 0:128], in_=negc[:], pattern=[[1, 128]],
                                               compare_op=ALU.is_ge, fill=0.0, base=0, channel_multiplier=-1), reads=[tC], writes=[tC])
        P.op("pool", lambda e: e.affine_select(out=tri2[:, 128:256], in_=negc[:], pattern=[[1, 128]],
                                               compare_op=ALU.is_gt, fill=0.0, base=0, channel_multiplier=-1), reads=[tC], writes=[tC])
        P.op("pool", lambda e: e.memset(blk1[:], 0.0), writes=[tC])
        P.op("pool", lambda e: e.memset(blk1[0:64, 0:64], 1.0), writes=[tC])
        P.op("pool", lambda e: e.memset(blk1[64:128, 64:128], 1.0), writes=[tC])
        P.op("pool", lambda e: e.memset(onesblk[:], 0.0), writes=[tC])
        P.op("pool", lambda e: e.memset(onesblk[0:64, 0:1], 1.0), writes=[tC])
        P.op("pool", lambda e: e.memset(onesblk[64:128, 1:2], 1.0), writes=[tC])
        P.dma("sp", cvS[:], cv, writes=[tC])
        P.dma("sp", lnS[:], ln.partition_broadcast(128), writes=[tC])
        P.op("dve", lambda e: e.tensor_scalar(out=omk[:], in0=cvS[:, 1, :], scalar1=-1.0, scalar2=1.0, op0=ALU.mult, op1=ALU.add),
             reads=[tC], writes=[tC])

        def bc8(ap2):
            return ap2.unsqueeze(2).to_broadcast([128, 8, 128])

        S = [sb(f"S{i}", [128, 8, 64], F32) for i in range(2)]
        tS = [Trk("S0"), Trk("S1")]
        P.op("pool", lambda e: e.memset(S[0][:], 0.0), writes=[tS[0]])
        Z = sb("Z", [128, 16, 2, 128], BF16); tZ = [Trk(f"Z{h}") for h in range(16)]
        Vz = sb("Vz", [128, 16, 128], BF16); tVz = Trk("Vz")
        PTbd = sb("PTbd", [128, 8, 128], F32); tPT = [Trk(f"PT{c}") for c in range(8)]
        P.op("pool", lambda e: e.memset(Z[:], 0.0), writes=tZ)
        P.op("pool", lambda e: e.memset(Vz[:], 0.0), writes=[tVz])
        P.op("pool", lambda e: e.memset(PTbd[:], 0.0), writes=tPT)
        Lv = [[sb(f"Lv{p}_{j}", [128, 384], BF16) for j in range(7)] for p in range(2)]
        tLv = [[Trk(f"Lv{p}_{j}") for j in range(7)] for p in range(2)]
        for p in range(2):
            P.op("pool", lambda e: e.tensor_copy(out=Lv[p][0][:, 128:256], in_=ident_f[:]), reads=[tC], writes=[tLv[p][0]])
        LR = [sb(f"LR{p}", [128, 384], BF16) for p in range(2)]; tLR = [Trk("LR0"), Trk("LR1")]
        TT = [sb(f"TT{p}", [128, 128], BF16) for p in range(2)]; tTT = [Trk("TT0"), Trk("TT1")]
        AW = sb("AW", [128, 16, 128], BF16); tAW = [Trk(f"AW{h}") for h in range(16)]
        fmS = [sb(f"fmS{i}", [128, 3, 8, 128], F32) for i in range(2)]; tfm = [Trk("fm0"), Trk("fm1")]
        tmS = [sb(f"tmS{i}", [128, 3, 1024], F32) for i in range(2)]; ttm = [Trk("tm0"), Trk("tm1")]
        E12 = sb("E12", [128, 8, 256], F32); tE12 = Trk("E12")
        E3 = sb("E3", [128, 8, 128], F32); tE3 = Trk("E3")
        E4 = sb("E4", [128, 8, 128], F32); tE4 = Trk("E4")
        Et = sb("Et", [128, 8, 128], F32); tEt = Trk("Et")
        mpos = sb("mpos", [128, 8], F32); negm = sb("negm", [128, 8], F32); cC = sb("cC", [128, 8], F32)
        em = sb("em", [128, 8], F32); gC = sb("gC", [128, 8], F32); tsm = Trk("small")
        kkraw = sb("kkraw", [128, 8, 128], F32); tkkraw = Trk()
        sq = sb("sq", [128, 8, 128], F32); tsq = Trk()
        rn = sb("rn", [128, 8, 128], F32); trn = Trk()
        kk = sb("kk", [128, 8, 128], F32); tkk = Trk()
        uu = sb("uu", [128, 8, 128], F32); tuu = Trk()
        kmod = sb("kmod", [128, 8, 128], F32); tkmod = Trk()
        beta = sb("beta", [128, 8, 128], F32); tbeta = Trk()
        RA = sb("RA", [128, 8, 256], BF16); tRA = Trk()
        BsT = sb("BsT", [128, 8, 128], BF16); tBsT = Trk()
        KsT = sb("KsT", [128, 8, 128], BF16); tKsT = Trk()
        BpF = sb("BpF", [128, 8, 128], BF16); tBpF = Trk()
        KpF = sb("KpF", [128, 8, 128], BF16); tKpF = Trk()
        RtT = sb("RtT", [128, 8, 128], F32); tRtT = Trk()
        prodT = sb("prodT", [128, 8, 128], BF16); tprod = Trk()
        BpT = sb("BpT", [128, 1024], BF16); tBpT = Trk()
        KpT = sb("KpT", [128, 1024], BF16); tKpT = Trk()
        DG = sb("DG", [128, 8, 64], F32); tDG = Trk()
        Qall = sb("Qall", [128, 8, 64], F32); tQ = Trk()
        RhT = sb("RhT", [128, 8, 128], F32); tRh = [Trk(f"Rh{c}") for c in range(8)]
        bon = sb("bon", [128, 16], F32); tbon = Trk()
        ysb = sb("ysb", [128, 16, 64], F32); tysb = Trk()
        ysq = sb("ysq", [128, 16, 64], F32); tysq = Trk()
        st1 = sb("st1", [128, 4, 16], F32); tst = Trk()
        yout = sb("yout", [128, 1024], out_dt); tyo = Trk()
        pc = ps("pc", [128, 2, 512]); tpc = [Trk("pc0"), Trk("pc1")]
        pt = ps("pt", [128, 1024], BF16); tpt = Trk("pt")
        ph = ps("ph", [128, 512]); tph = Trk("ph")
        pi = [ps(f"pi{i}", [128, 512]) for i in range(2)]; tpi = [Trk("pi0"), Trk("pi1")]
        pq = ps("pq", [128, 512]); tpq = Trk("pq")
        pS = ps("pS", [128, 8, 64]); tpS = Trk("pS")

        def load(n):
            b = n % 2
            P.dma("sp", fmS[b][:], fm[n], writes=[tfm[b]])
            P.dma("act", tmS[b][:], tm[n], writes=[ttm[b]])

        load(0)
        for n in range(NCH):
            b = n % 2
            if n + 1 < NCH:
                load(n + 1)
            R_ = fmS[b][:, 0]; K_ = fmS[b][:, 1]; A_ = fmS[b][:, 2]
            V_ = tmS[b][:, 0, :]; GS_ = tmS[b][:, 1, :]; SG_ = tmS[b][:, 2, :]
            Sp, Sn = S[b], S[1 - b]; tSp, tSn = tS[b], tS[1 - b]
            for half in range(2):
                for j in range(4):
                    cg = half * 4 + j
                    P.op("pe", lambda e: e.matmul(pc[:, j // 2, (j % 2) * 256:(j % 2) * 256 + 256], SG_[:, cg * 128:(cg + 1) * 128], tri2[:],
                                                  start=True, stop=True),
                         reads=[ttm[b], tC], writes=[tpc[j // 2]], same_eng_raw=False)
                pcv = pc[:].rearrange("p a (b c) -> p (a b) c", b=2)
                sl = slice(half * 4, half * 4 + 4)
                P.op("dve", lambda e: e.tensor_copy(out=mpos[:, sl], in_=pcv[:, :, 63]), reads=tpc, writes=[tsm])
                P.op("dve", lambda e: e.tensor_scalar(out=negm[:, sl], in0=pcv[:, :, 63], scalar1=-1.0, scalar2=None, op0=ALU.mult), reads=tpc, writes=[tsm])
                P.op("dve", lambda e: e.tensor_copy(out=cC[:, sl], in_=pcv[:, :, 127]), reads=tpc, writes=[tsm])
                for j in range(4):
                    cg = half * 4 + j
                    P.op("act", lambda e: e.activation(out=E12[:, cg, :], in_=pcv[:, j, :], func=AF.Exp, bias=negm[:, cg:cg + 1], scale=1.0),
                         reads=tpc + [tsm], writes=[tE12])
                    P.op("act", lambda e: e.activation(out=E3[:, cg, :], in_=pcv[:, j, 0:128], func=AF.Exp, bias=mpos[:, cg:cg + 1], scale=-1.0),
                         reads=tpc + [tsm], writes=[tE3])
                    P.op("act", lambda e: e.activation(out=E4[:, cg, :], in_=pcv[:, j, 0:128], func=AF.Exp, bias=cC[:, cg:cg + 1], scale=-1.0),
                         reads=tpc + [tsm], writes=[tE4])
                    P.op("act", lambda e: e.activation(out=Et[:, cg, :], in_=pcv[:, j, 0:128], func=AF.Exp),
                         reads=tpc + [tsm], writes=[tEt])
            P.op("act", lambda e: e.activation(out=em[:], in_=mpos[:], func=AF.Exp), reads=[tsm], writes=[tsm])
            P.op("act", lambda e: e.activation(out=gC[:], in_=cC[:], func=AF.Exp), reads=[tsm], writes=[tsm])
            P.op("dve", lambda e: e.tensor_tensor(out=DG[:], in0=idpair[:].unsqueeze(1).to_broadcast([128, 8, 64]),
                                                  in1=gC[:].unsqueeze(2).to_broadcast([128, 8, 64]), op=ALU.mult), reads=[tsm, tC], writes=[tDG])
            P.op("pool", lambda e: e.tensor_tensor(out=kkraw[:], in0=K_, in1=bc8(cvS[:, 0, :]), op=ALU.mult), reads=[tfm[b], tC], writes=[tkkraw])
            P.op("pool", lambda e: e.tensor_tensor(out=sq[:], in0=kkraw[:], in1=kkraw[:], op=ALU.mult), reads=[tkkraw], writes=[tsq])
            for cg in range(8):
                P.op("pe", lambda e: e.matmul(pc[:, cg // 4, (cg % 4) * 128:(cg % 4) * 128 + 128], blk1[:], sq[:, cg, :], start=True, stop=True),
                     reads=[tsq, tC], writes=[tpc[cg // 4]], same_eng_raw=False)
            P.op("dve", lambda e: e.tensor_scalar(out=rn[:], in0=pc[:].rearrange("p a (b c) -> p (a b) c", b=4), scalar1=1e-24, scalar2=None,
                                                  op0=ALU.max), reads=tpc, writes=[trn])
            P.op("act", lambda e: e.activation(out=rn[:], in_=rn[:], func=AF.Sqrt), reads=[trn], writes=[trn])
            P.op("dve", lambda e: e.reciprocal(out=rn[:], in_=rn[:]), reads=[trn], writes=[trn])
            P.op("dve", lambda e: e.tensor_tensor(out=kk[:], in0=kkraw[:], in1=rn[:], op=ALU.mult), reads=[tkkraw, trn], writes=[tkk])
            P.op("pool", lambda e: e.tensor_tensor(out=uu[:], in0=A_, in1=bc8(cvS[:, 1, :]), op=ALU.mult), reads=[tfm[b], tC], writes=[tuu])
            P.op("pool", lambda e: e.tensor_tensor(out=uu[:], in0=uu[:], in1=bc8(omk[:]), op=ALU.add), reads=[tuu, tC], writes=[tuu])
            P.op("pool", lambda e: e.tensor_tensor(out=kmod[:], in0=K_, in1=uu[:], op=ALU.mult), reads=[tfm[b], tuu], writes=[tkmod])
            P.op("pool", lambda e: e.tensor_tensor(out=beta[:], in0=kk[:], in1=A_, op=ALU.mult), reads=[tkk, tfm[b]], writes=[tbeta])
            P.op("dve", lambda e: e.scalar_tensor_tensor(out=RA[:, :, 0:128], in0=kk[:], scalar=-1.0, in1=E12[:, :, 128:256], op0=ALU.mult, op1=ALU.mult),
                 reads=[tkk, tE12], writes=[tRA])
            P.op("pool", lambda e: e.tensor_tensor(out=RA[:, :, 128:256], in0=R_, in1=E12[:, :, 0:128], op=ALU.mult), reads=[tfm[b], tE12], writes=[tRA])
            P.op("dve", lambda e: e.tensor_tensor(out=BsT[:], in0=beta[:], in1=E3[:], op=ALU.mult), reads=[tbeta, tE3], writes=[tBsT])
            P.op("pool", lambda e: e.tensor_tensor(out=KsT[:], in0=kmod[:], in1=E3[:], op=ALU.mult), reads=[tkmod, tE3], writes=[tKsT])
            P.op("dve", lambda e: e.tensor_tensor(out=BpF[:], in0=beta[:], in1=E4[:], op=ALU.mult), reads=[tbeta, tE4], writes=[tBpF])
            P.op("pool", lambda e: e.tensor_tensor(out=KpF[:], in0=kmod[:], in1=E4[:], op=ALU.mult), reads=[tkmod, tE4], writes=[tKpF])
            P.op("pool", lambda e: e.tensor_tensor(out=RtT[:], in0=R_, in1=Et[:], op=ALU.mult), reads=[tfm[b], tEt], writes=[tRtT])
            P.op("pool", lambda e: e.tensor_tensor(out=uu[:], in0=R_, in1=kmod[:], op=ALU.mult), reads=[tfm[b], tkmod], writes=[tuu])
            P.op("pool", lambda e: e.tensor_tensor(out=prodT[:], in0=uu[:], in1=bc8(cvS[:, 2, :]), op=ALU.mult), reads=[tuu, tC], writes=[tprod])
            V4 = V_.rearrange("p (c h v) -> p c h v", h=2, v=64)
            Vz4 = Vz[:].rearrange("p (c h) x -> p c h x", h=2)
            P.op("act", lambda e: e.copy(out=Vz4[:, :, 0, 0:64], in_=V4[:, :, 0, :]), reads=[ttm[b]], writes=[tVz])
            P.op("act", lambda e: e.copy(out=Vz4[:, :, 1, 64:128], in_=V4[:, :, 1, :]), reads=[ttm[b]], writes=[tVz])
            for cg in range(8):
                P.op("pe", lambda e: e.matmul(pq[:, 384 + 2 * cg:386 + 2 * cg], prodT[:, cg, :], onesblk[:], start=True, stop=True),
                     reads=[tprod, tC], writes=[tpq], same_eng_raw=False)
            P.op("dve", lambda e: e.tensor_copy(out=bon[:], in_=pq[:, 384:400]), reads=[tpq], writes=[tbon])
            for kind in range(3):
                src_t, trk = [(RA, tRA), (BpF, tBpF), (KpF, tKpF)][kind]
                for cg in range(8):
                    P.op("pe", lambda e: e.transpose(pt[:, cg * 128:(cg + 1) * 128], src_t[:, cg, 0:128], ident_b[:]),
                         reads=[trk, tC], writes=[tpt], same_eng_raw=False)
                if kind == 0:
                    P.op("dve", lambda e: e.tensor_copy(out=AW[:, :, 0:64], in_=pt[:].rearrange("p (h x) -> p h x", x=64)), reads=[tpt], writes=tAW)
                elif kind == 1:
                    P.op("act", lambda e: e.copy(out=BpT[:], in_=pt[:]), reads=[tpt], writes=[tBpT])
                else:
                    P.op("dve", lambda e: e.tensor_copy(out=KpT[:], in_=pt[:]), reads=[tpt], writes=[tKpT])
            for cg in range(8):
                for hh in range(2):
                    h = 2 * cg + hh
                    hs = slice(hh * 64, hh * 64 + 64)
                    L = Lv[hh]; tL = tLv[hh]
                    P.op("pe", lambda e: e.matmul(ph[:, 0:256], BsT[hs, cg, :], RA[hs, cg, :], start=True, stop=True),
                         reads=[tBsT, tRA], writes=[tph], same_eng_raw=False)
                    P.op("pe", lambda e: e.matmul(ph[:, 256:512], KsT[hs, cg, :], RA[hs, cg, :], start=True, stop=True),
                         reads=[tKsT, tRA], writes=[tph], same_eng_raw=False)
                    P.op("pe", lambda e: e.matmul(pi[0][:, 384:512], RA[hs, cg, 0:128], BsT[hs, cg, :], start=True, stop=True),
                         reads=[tBsT, tRA], writes=[tpi[0]], same_eng_raw=False)
                    P.op("dve", lambda e: e.tensor_tensor(out=L[0][:, 0:128], in0=ph[:, 0:128], in1=masku4[:, 0:128], op=ALU.mult),
                         reads=[tph, tC], writes=[tL[0]])
                    P.op("dve", lambda e: e.tensor_tensor(out=LR[hh][:], in0=ph[:, 128:512], in1=masku4[:, 128:512], op=ALU.mult),
                         reads=[tph, tC], writes=[tLR[hh]])
                    P.op("dve", lambda e: e.tensor_tensor(out=L[0][:, 256:384], in0=pi[0][:, 384:512], in1=maskl[:], op=ALU.mult),
                         reads=[tpi[0], tC], writes=[tL[0]])
                    for j in range(6):
                        pp_, tpp = pi[j % 2], tpi[j % 2]
                        s_, d_ = L[j], L[j + 1]
                        P.op("pe", lambda e: e.matmul(pp_[:, 0:256], s_[:, 256:384], s_[:, 0:256], start=True, stop=False),
                             reads=[tL[j]], writes=[tpp], same_eng_raw=False)
                        P.op("pe", lambda e: e.matmul(pp_[:, 128:256], ident_b[:], s_[:, 128:256], start=False, stop=True),
                             reads=[tL[j], tC], writes=[tpp], same_eng_raw=False)
                        P.op("pe", lambda e: e.matmul(pp_[:, 256:384], s_[:, 0:128], s_[:, 256:384], start=True, stop=True),
                             reads=[tL[j]], writes=[tpp], same_eng_raw=False)
                        if j % 2 == 0:
                            P.op("act", lambda e: e.copy(out=d_[:], in_=pp_[:, 0:384]), reads=[tpp], writes=[tL[j + 1]])
                        else:
                            P.op("dve", lambda e: e.tensor_copy(out=d_[:], in_=pp_[:, 0:384]), reads=[tpp], writes=[tL[j + 1]])
                    P.op("pe", lambda e: e.matmul(pi[0][:, 0:128], L[6][:, 256:384], L[6][:, 128:256], start=True, stop=False),
                         reads=[tL[6]], writes=[tpi[0]], same_eng_raw=False)
                    P.op("pe", lambda e: e.matmul(pi[0][:, 0:128], ident_b[:], L[6][:, 128:256], start=False, stop=True),
                         reads=[tL[6], tC], writes=[tpi[0]], same_eng_raw=False)
                    P.op("act", lambda e: e.copy(out=TT[hh][:], in_=pi[0][:, 0:128]), reads=[tpi[0]], writes=[tTT[hh]])
                    P.op("pe", lambda e: e.matmul(pi[1][:, 0:64], LR[hh][:, 128:256], Vz[:, h, hs], start=True, stop=True),
                         reads=[tLR[hh], tVz], writes=[tpi[1]], same_eng_raw=False)
                    P.op("dve", lambda e: e.tensor_copy(out=AW[:, h, 64:128], in_=pi[1][:, 0:64]), reads=[tpi[1]], writes=[tAW[h]])
                    P.op("pe", lambda e: e.matmul(pi[1][:, 128:256], TT[hh][:], AW[:, h, :], start=True, stop=True),
                         reads=[tTT[hh], tAW[h]], writes=[tpi[1]], same_eng_raw=False)
                    P.op("act", lambda e: e.copy(out=Z[:, h, :, hs], in_=pi[1][:, 128:256].rearrange("p (a b) -> p a b", a=2)),
                         reads=[tpi[1]], writes=[tZ[h]])
                h0, h1 = 2 * cg, 2 * cg + 1
                csl = slice(cg * 128, cg * 128 + 128)
                P.op("pe", lambda e: e.matmul(pq[:, 0:128], Z[:, h0, 0, :], LR[0][:, 0:128], start=True, stop=False),
                     reads=[tZ[h0], tLR[0]], writes=[tpq], same_eng_raw=False)
                P.op("pe", lambda e: e.matmul(pq[:, 0:128], Z[:, h1, 0, :], LR[1][:, 0:128], start=False, stop=True),
                     reads=[tZ[h1], tLR[1]], writes=[tpq], same_eng_raw=False)
                P.op("dve", lambda e: e.scalar_tensor_tensor(out=RhT[:, cg, :], in0=pq[:, 0:128], scalar=em[:, cg:cg + 1], in1=RtT[:, cg, :],
                                                             op0=ALU.mult, op1=ALU.add), reads=[tpq, tsm, tRtT], writes=[tRh[cg]])
                P.op("pe", lambda e: e.matmul(pq[:, 128:256], Z[:, h0, 0, :], BpT[:, csl], start=True, stop=False),
                     reads=[tZ[h0], tBpT], writes=[tpq], same_eng_raw=False)
                P.op("pe", lambda e: e.matmul(pq[:, 128:256], Z[:, h1, 0, :], BpT[:, csl], start=False, stop=True),
                     reads=[tZ[h1], tBpT], writes=[tpq], same_eng_raw=False)
                for hh in range(2):
                    hs = slice(hh * 64, hh * 64 + 64)
                    P.op("dve", lambda e: e.scalar_tensor_tensor(out=PTbd[hs, cg, hs], in0=pq[hs, 128 + hh * 64:192 + hh * 64], scalar=em[hs, cg:cg + 1],
                                                                 in1=DG[hs, cg, :], op0=ALU.mult, op1=ALU.add),
                         reads=[tpq, tsm, tDG], writes=[tPT[cg]])
                P.op("pe", lambda e: e.matmul(pq[:, 256:512].rearrange("p (a b) -> p a b", a=2), BpT[:, csl], Z[:, h0:h0 + 2, 1, :], start=True, stop=False),
                     reads=[tZ[h0], tZ[h1], tBpT], writes=[tpq], same_eng_raw=False)
                P.op("pe", lambda e: e.matmul(pq[:, 256:512].rearrange("p (a b) -> p a b", a=2), KpT[:, csl], Vz[:, h0:h0 + 2, :], start=False, stop=True),
                     reads=[tKpT, tVz], writes=[tpq], same_eng_raw=False)
                P.op("act", lambda e: e.copy(out=Qall[0:64, cg, :], in_=pq[0:64, 256:320]), reads=[tpq], writes=[tQ])
                P.op("act", lambda e: e.copy(out=Qall[64:128, cg, :], in_=pq[64:128, 448:512]), reads=[tpq], writes=[tQ])
                for hh in range(2):
                    h = 2 * cg + hh
                    hs = slice(hh * 64, hh * 64 + 64)
                    yv = pc[:, h // 8, (h % 8) * 64:(h % 8) * 64 + 64]
                    P.op("pe", lambda e: e.matmul(yv, LR[hh][:, 0:128], Z[:, h, 1, hs], start=True, stop=False),
                         reads=[tLR[hh], tZ[h]], writes=[tpc[h // 8]], same_eng_raw=False)
                    P.op("pe", lambda e: e.matmul(yv, LR[hh][:, 256:384], Vz[:, h, hs], start=False, stop=False),
                         reads=[tLR[hh], tVz], writes=[tpc[h // 8]], same_eng_raw=False)
                    P.op("pe", lambda e: e.matmul(yv, RhT[hs, cg, :], Sp[hs, cg, :], start=False, stop=True),
                         reads=[tRh[cg], tSp], writes=[tpc[h // 8]], same_eng_raw=False)
                P.op("pe", lambda e: e.matmul(pS[:, cg, :], PTbd[:, cg, :], Sp[:, cg, :], start=True, stop=True),
                     reads=[tPT[cg], tSp], writes=[tpS], same_eng_raw=False)
            P.op("dve", lambda e: e.tensor_tensor(out=Sn[:], in0=pS[:], in1=Qall[:], op=ALU.add), reads=[tpS, tQ], writes=[tSn])
            for a in range(2):
                P.op("act", lambda e: e.copy(out=ysb[:, a * 8:(a + 1) * 8, :], in_=pc[:, a, :].rearrange("p (h v) -> p h v", v=64)),
                     reads=[tpc[a]], writes=[tysb])
            P.op("dve", lambda e: e.tensor_reduce(out=st1[:, 0, :], in_=ysb[:], axis=AX.X, op=ALU.add), reads=[tysb], writes=[tst])
            P.op("pool", lambda e: e.tensor_tensor(out=ysq[:], in0=ysb[:], in1=ysb[:], op=ALU.mult), reads=[tysb], writes=[tysq])
            P.op("dve", lambda e: e.tensor_reduce(out=st1[:, 1, :], in_=ysq[:], axis=AX.X, op=ALU.add), reads=[tysq], writes=[tst])
            P.op("dve", lambda e: e.tensor_scalar(out=st1[:, 0, :], in0=st1[:, 0, :], scalar1=1.0 / 64, scalar2=None, op0=ALU.mult), reads=[tst], writes=[tst])
            P.op("dve", lambda e: e.tensor_tensor(out=st1[:, 2, :], in0=st1[:, 0, :], in1=st1[:, 0, :], op=ALU.mult), reads=[tst], writes=[tst])
            P.op("dve", lambda e: e.scalar_tensor_tensor(out=st1[:, 1, :], in0=st1[:, 1, :], scalar=1.0 / 64, in1=st1[:, 2, :], op0=ALU.mult, op1=ALU.subtract),
                 reads=[tst], writes=[tst])
            P.op("dve", lambda e: e.tensor_scalar(out=st1[:, 3, :], in0=st1[:, 1, :], scalar1=GN_EPS, scalar2=None, op0=ALU.add), reads=[tst], writes=[tst])
            P.op("act", lambda e: e.activation(out=st1[:, 3, :], in_=st1[:, 3, :], func=AF.Sqrt), reads=[tst], writes=[tst])
            P.op("dve", lambda e: e.reciprocal(out=st1[:, 3, :], in_=st1[:, 3, :]), reads=[tst], writes=[tst])

            def bc16(ap2):
                return ap2.unsqueeze(2).to_broadcast([128, 16, 64])
            P.op("dve", lambda e: e.tensor_tensor(out=ysb[:], in0=ysb[:], in1=bc16(st1[:, 0, :]), op=ALU.subtract), reads=[tysb, tst], writes=[tysb])
            P.op("pool", lambda e: e.tensor_tensor(out=ysb[:], in0=ysb[:], in1=bc16(st1[:, 3, :]), op=ALU.mult), reads=[tysb, tst], writes=[tysb])
            ysf = ysb[:].rearrange("p h v -> p (h v)")
            P.op("dve", lambda e: e.tensor_tensor(out=ysf, in0=ysf, in1=lnS[:, 0, :], op=ALU.mult), reads=[tysb, tC], writes=[tysb])
            P.op("pool", lambda e: e.tensor_tensor(out=ysf, in0=ysf, in1=lnS[:, 1, :], op=ALU.add), reads=[tysb, tC], writes=[tysb])
            P.op("pool", lambda e: e.tensor_tensor(out=ysq[:], in0=V_.rearrange("p (h v) -> p h v", v=64), in1=bc16(bon[:]), op=ALU.mult),
                 reads=[ttm[b], tbon], writes=[tysq])
            P.op("dve", lambda e: e.tensor_tensor(out=ysb[:], in0=ysb[:], in1=ysq[:], op=ALU.add), reads=[tysb, tysq], writes=[tysb])
            P.op("pool", lambda e: e.tensor_tensor(out=yout[:], in0=ysf, in1=GS_, op=ALU.mult), reads=[tysb, ttm[b]], writes=[tyo])
            P.dma("sp", yo[n], yout[:], reads=[tyo])
        P.wait_all("sp", [tyo])
        print("scan insts", P.n_inst)
    return nc


NORM_EPS = 1e-6
TOK = 2048


class Stage:
    def __init__(self, name):
        self.nc = bass.Bass("TRN2", target_bir_lowering=False)
        self.st = contextlib.ExitStack()
        self.P = Prog(self.nc, self.st)
        P = self.P
        self.AT = None; self.tAT = Trk("AT")
        self.wring = [P.sb(f"wt{i}", [128, 32, 128], BF16) for i in range(3)]; self.twr = [Trk(f"wt{i}") for i in range(3)]
        self.wi = 0
        self.pg = [P.ps(f"pg{i}", [128, 512]) for i in range(4)]; self.tpg = [Trk(f"pg{i}") for i in range(4)]
        self.pgi = 0
        self.ssum = P.ps("ssum", [128, 4, 512]); self.tss = [Trk(f"ss{i}") for i in range(4)]
        self.stg = [P.sb(f"stg{i}", [128, 512], F32) for i in range(4)]; self.tstg = [Trk(f"stg{i}") for i in range(4)]
        self.stb = [P.sb(f"stb{i}", [128, 512], BF16) for i in range(4)]; self.tstb = [Trk(f"stb{i}") for i in range(4)]
        self.si = 0
        self.ones_b = P.sb("ones_b", [128, 128], BF16); self.tC = Trk("C")
        P.op("pool", lambda e: e.memset(self.ones_b[:], 1.0), writes=[self.tC])
        self.rstd = P.sb("rstd", [128, TOK], F32); self.trstd = Trk("rstd")

    def alloc_AT(self, fence=()):
        self.AT = self.P.sb("AT", [128, 32, TOK], BF16)
        self.P.wait_all("sp", list(fence))

    def tmp_stack(self):
        st2 = contextlib.ExitStack()
        return st2

    def din(self, name, shape, dt=F32):
        return self.nc.dram_tensor(name, list(shape), dt, kind="ExternalInput").ap()

    def dout(self, name, shape, dt=F32):
        return self.nc.dram_tensor(name, list(shape), dt, kind="ExternalOutput").ap()

    def dscr(self, name, shape, dt=F32):
        return self.nc.dram_tensor(name, list(shape), dt, kind="Internal").ap()

    def stage(self, dt=F32):
        i = self.si % 4
        self.si += 1
        return (self.stg[i], self.tstg[i]) if dt == F32 else (self.stb[i], self.tstb[i])

    def gemm(self, W, n0, N, KC, epi, AT=None, tAT=None):
        P = self.P
        AT = self.AT if AT is None else AT
        tAT = self.tAT if tAT is None else tAT
        for j in range(N // 128):
            wt, twt = self.wring[self.wi % 3], self.twr[self.wi % 3]
            self.wi += 1
            src = W[:, n0 + j * 128:n0 + (j + 1) * 128]
            if KC > 1:
                src = src.rearrange("(c p) n -> p c n", p=128)
                P.dma("pool", wt[:, 0:KC, :], src, writes=[twt])
            else:
                P.dma("pool", wt[:, 0, :], src, writes=[twt])
            for tb in range(TOK // 512):
                pb, tpb = self.pg[self.pgi % 4], self.tpg[self.pgi % 4]
                self.pgi += 1
                for c in range(KC):
                    P.op("pe", lambda e: e.matmul(pb[:], wt[:, c, :], AT[:, c, tb * 512:(tb + 1) * 512], start=(c == 0), stop=(c == KC - 1)),
                         reads=[twt, tAT], writes=[tpb], same_eng_raw=False)
                epi(j, tb, pb, tpb)

    def sumsq_acc(self, src_ap, reads, tb, first, last):
        P = self.P
        sq, tsq = self.stage(BF16)
        P.op("act", lambda e: e.activation(out=sq[:], in_=src_ap, func=AF.Square), reads=reads, writes=[tsq])
        P.op("pe", lambda e: e.matmul(self.ssum[:, tb, :], self.ones_b[:], sq[:], start=first, stop=last),
             reads=[tsq, self.tC], writes=[self.tss[tb]], same_eng_raw=False)

    def make_rstd(self):
        P = self.P
        for tb in range(4):
            sl = slice(tb * 512, (tb + 1) * 512)
            P.op("dve", lambda e: e.tensor_scalar(out=self.rstd[:, sl], in0=self.ssum[:, tb, :], scalar1=1.0 / 4096, scalar2=NORM_EPS,
                                                  op0=ALU.mult, op1=ALU.add), reads=[self.tss[tb]], writes=[self.trstd])
        P.op("act", lambda e: e.activation(out=self.rstd[:], in_=self.rstd[:], func=AF.Sqrt), reads=[self.trstd], writes=[self.trstd])
        P.op("dve", lambda e: e.reciprocal(out=self.rstd[:], in_=self.rstd[:]), reads=[self.trstd], writes=[self.trstd])

    def finish(self, trks):
        self.P.wait_everything("sp")
        self.st.close()
        return self.nc


def build_A1():
    S = Stage("A1"); P = S.P
    xT = S.din("xT", [4096, TOK]); xp = S.din("xp", [128, 32])
    vec = S.din("vec", [128, 9, 32])
    w_in = S.din("w_in", [4, 4096, 4096]); w1 = S.din("w1", [4096, 128]); w2 = S.din("w2", [128, 4096])
    a1 = S.din("a1", [4096, 128]); a2 = S.din("a2", [128, 4096])
    outs = [S.dout(n, [4096, TOK]) for n in ("rT", "kT", "vT", "gsT", "sgT", "aT")]
    lerp = [S.dscr(f"lerp{i}", [4096, TOK], BF16) for i in range(6)]
    vS = P.sb("vS", [128, 9, 32], F32); tv = Trk("vec")
    P.dma("sp", vS[:], vec, writes=[tv])
    xpS = P.sb("xpS", [128, 32], F32); hnp = P.sb("hnp", [128, 32], F32); tmp32 = P.sb("tmp32", [128, 32], BF16); txp = Trk("xp")
    P.dma("sp", xpS[:], xp, writes=[txp])
    P.op("act", lambda e: e.activation(out=tmp32[:], in_=xpS[:], func=AF.Square), reads=[txp], writes=[txp])
    P.op("pe", lambda e: e.matmul(S.pg[0][:, 0:32], S.ones_b[:], tmp32[:], start=True, stop=True), reads=[txp, S.tC], writes=[S.tpg[0]], same_eng_raw=False)
    rp = P.sb("rp", [128, 1], F32)
    P.op("dve", lambda e: e.tensor_reduce(out=rp[:], in_=S.pg[0][:, 0:32], axis=AX.X, op=ALU.add), reads=[S.tpg[0]], writes=[txp])
    P.op("dve", lambda e: e.tensor_scalar(out=rp[:], in0=rp[:], scalar1=1.0 / 4096, scalar2=NORM_EPS, op0=ALU.mult, op1=ALU.add), reads=[txp], writes=[txp])
    P.op("act", lambda e: e.activation(out=rp[:], in_=rp[:], func=AF.Sqrt), reads=[txp], writes=[txp])
    P.op("dve", lambda e: e.reciprocal(out=rp[:], in_=rp[:]), reads=[txp], writes=[txp])
    P.op("dve", lambda e: e.scalar_tensor_tensor(out=hnp[:], in0=xpS[:], scalar=rp[:, 0:1], in1=vS[:, 0, :], op0=ALU.mult, op1=ALU.mult), reads=[txp, tv], writes=[txp])
    st2 = contextlib.ExitStack()
    sb2 = lambda name, shape, dt: st2.enter_context(S.nc.sbuf_tensor(name, list(shape), dt))
    xc = [sb2(f"xc{i}", [128, TOK], F32) for i in range(2)]; txc = [Trk("xc0"), Trk("xc1")]
    for c in range(32):
        b = c % 2
        P.dma("sp", xc[b][:], xT[c * 128:(c + 1) * 128, :], writes=[txc[b]])
        for tb in range(4):
            S.sumsq_acc(xc[b][:, tb * 512:(tb + 1) * 512], [txc[b]], tb, c == 0, c == 31)
    S.make_rstd()
    hn = sb2("hn", [128, TOK], F32); thn = Trk("hn")
    dx = sb2("dx", [128, TOK], F32); tdx = Trk("dx")
    lo = [sb2(f"lo{i}", [128, TOK], BF16) for i in range(3)]; tlo = [Trk(f"lo{i}") for i in range(3)]
    li = 0
    for c in range(32):
        b = c % 2
        P.dma("sp", xc[b][:], xT[c * 128:(c + 1) * 128, :], writes=[txc[b]])
        P.op("dve", lambda e: e.scalar_tensor_tensor(out=hn[:], in0=xc[b][:], scalar=vS[:, 0, c:c + 1], in1=S.rstd[:], op0=ALU.mult, op1=ALU.mult),
             reads=[txc[b], tv, S.trstd], writes=[thn])
        P.op("pool", lambda e: e.tensor_tensor(out=dx[:, 1:TOK], in0=hn[:, 0:TOK - 1], in1=hn[:, 1:TOK], op=ALU.subtract), reads=[thn], writes=[tdx])
        P.op("pool", lambda e: e.tensor_tensor(out=dx[:, 0:1], in0=hnp[:, c:c + 1], in1=hn[:, 0:1], op=ALU.subtract), reads=[thn, txp], writes=[tdx])
        for i in range(6):
            l_, tl_ = lo[li % 3], tlo[li % 3]; li += 1
            eng = "dve"
            P.op(eng, lambda e: e.scalar_tensor_tensor(out=l_[:], in0=dx[:], scalar=vS[:, 1 + i, c:c + 1], in1=hn[:], op0=ALU.mult, op1=ALU.add),
                 reads=[tdx, thn, tv], writes=[tl_])
            P.dma("act", lerp[i][c * 128:(c + 1) * 128, :], l_[:], reads=[tl_])
    tlerp_done = tlo
    st2.close()
    S.alloc_AT(fence=txc + [thn, tdx] + tlo)
    hw = P.sb("hw", [128, 1, TOK], BF16); thw = Trk("hw")

    def load_AT(i):
        for q in range(4):
            P.dma("sp", S.AT[:, q * 8:(q + 1) * 8, :], lerp[i][q * 1024:(q + 1) * 1024, :].rearrange("(c p) t -> p c t", p=128),
                  reads=tlerp_done, writes=[S.tAT])

    def epi_out(out_ap, func):
        def f(j, tb, pb, tpb):
            s_, ts_ = S.stage(F32)
            P.op("act", lambda e: e.activation(out=s_[:], in_=pb[:], func=func), reads=[tpb], writes=[ts_])
            P.dma("sp", out_ap[j * 128:(j + 1) * 128, tb * 512:(tb + 1) * 512], s_[:], reads=[ts_])
        return f

    def epi_hw(func):
        def f(j, tb, pb, tpb):
            P.op("act", lambda e: e.activation(out=hw[:, 0, tb * 512:(tb + 1) * 512], in_=pb[:], func=func), reads=[tpb], writes=[thw])
        return f

    def epi_sig(out_ap, kind):
        def f(j, tb, pb, tpb):
            s_, ts_ = S.stage(F32)
            P.op("act", lambda e: e.activation(out=s_[:], in_=pb[:], func=AF.Sigmoid, bias=vS[:, kind, j:j + 1], scale=1.0), reads=[tpb, tv], writes=[ts_])
            P.dma("sp", out_ap[j * 128:(j + 1) * 128, tb * 512:(tb + 1) * 512], s_[:], reads=[ts_])
        return f

    for i in range(4):
        load_AT(i)
        S.gemm(w_in[i], 0, 4096, 32, epi_out(outs[i], AF.Silu if i == 3 else AF.Copy))
    load_AT(4)
    S.gemm(w1, 0, 128, 32, epi_hw(AF.Tanh))
    S.gemm(w2, 0, 4096, 1, epi_sig(outs[4], 7), AT=hw, tAT=thw)
    load_AT(5)
    S.gemm(a1, 0, 128, 32, epi_hw(AF.Copy))
    S.gemm(a2, 0, 4096, 1, epi_sig(outs[5], 8), AT=hw, tAT=thw)
    print("A1 insts", P.n_inst)
    return S.finish(S.tstg)


def wo_norm_res(S, AsrcT, resT, w_o, gcol, outT, tout, mix, cast_load=True, stop=99, out2=None):
    P = S.P
    for q in range(4):
        P.dma("pool", S.AT[:, q * 8:(q + 1) * 8, :], AsrcT[q * 1024:(q + 1) * 1024, :].rearrange("(c p) t -> p c t", p=128), writes=[S.tAT])
    tmix = [Trk(f"mix{j}") for j in range(32)]

    def epi(j, tb, pb, tpb):
        s_, ts_ = S.stage(F32)
        P.op("act", lambda e: e.copy(out=s_[:], in_=pb[:]), reads=[tpb], writes=[ts_])
        P.dma("sp", mix[j * 128:(j + 1) * 128, tb * 512:(tb + 1) * 512], s_[:], reads=[ts_], writes=[tmix[j]])
        S.sumsq_acc(pb[:], [tpb], tb, j == 0, j == 31)
    if stop <= 0:
        return
    S.gemm(w_o, 0, 4096, 32, epi)
    S.make_rstd()
    if stop <= 1:
        return
    for j in range(32):
        b = j % 2
        P.dma("sp", S.mc[b][:], mix[j * 128:(j + 1) * 128, :], reads=[tmix[j]], writes=[S.tmc[b]])
        P.dma("act", S.xc[b][:], resT[j * 128:(j + 1) * 128, :], writes=[S.txc[b]])
        P.op("dve", lambda e: e.scalar_tensor_tensor(out=S.mc[b][:], in0=S.mc[b][:], scalar=gcol(j), in1=S.rstd[:], op0=ALU.mult, op1=ALU.mult),
             reads=[S.tmc[b], S.trstd, S.tv], writes=[S.tmc[b]])
        P.op("pool", lambda e: e.tensor_tensor(out=S.mc[b][:], in0=S.mc[b][:], in1=S.xc[b][:], op=ALU.add), reads=[S.tmc[b], S.txc[b]], writes=[S.tmc[b]])
        P.dma("sp", outT[j * 128:(j + 1) * 128, :], S.mc[b][:], reads=[S.tmc[b]], writes=[tout[j]])
        if out2 is not None:
            P.dma("act", out2[j * 128:(j + 1) * 128, :], S.mc[b][:], reads=[S.tmc[b]], writes=[tout[j]])
        yield j, S.mc[b], S.tmc[b]


def alloc_chunks(S):
    P = S.P
    S.mc = [P.sb(f"mc{i}", [128, TOK], F32) for i in range(2)]; S.tmc = [Trk("mc0"), Trk("mc1")]
    S.xc = [P.sb(f"xcb{i}", [128, TOK], F32) for i in range(2)]; S.txc = [Trk("xcb0"), Trk("xcb1")]


def build_B(stop=99):
    S = Stage("B"); P = S.P
    yT = S.din("yT", [4096, TOK]); xT = S.din("xT", [4096, TOK]); vec = S.din("vec", [128, 3, 32])
    w_o = S.din("w_o", [4096, 4096]); w_k = S.din("w_k", [4096, 4096]); w_v = S.din("w_v", [4096, 4096]); w_qg = S.din("w_qg", [4096, 8192])
    h1T = S.dout("h1T", [4096, TOK]); kT = S.dout("kT", [4096, TOK]); vT = S.dout("vT", [4096, TOK])
    qT = S.dout("qT", [4096, TOK]); gsT = S.dout("gsT", [4096, TOK]); kmT = S.dout("kmT", [4096, 8])
    mix = S.dscr("mix", [4096, TOK]); h1s = S.dscr("h1s", [4096, TOK])
    S.vS = P.sb("vS", [128, 3, 32], F32); S.tv = Trk("vec")
    P.dma("sp", S.vS[:], vec, writes=[S.tv])
    alloc_chunks(S)
    S.alloc_AT()
    th1 = [Trk(f"h1_{j}") for j in range(32)]
    for j, h1c, th in wo_norm_res(S, yT, xT, w_o, lambda j: S.vS[:, 0, j:j + 1], h1T, th1, mix, stop=stop, out2=h1s):
        for tb in range(4):
            S.sumsq_acc(h1c[:, tb * 512:(tb + 1) * 512], [th], tb, j == 0, j == 31)
    if stop <= 2:
        return S.finish([])
    S.make_rstd()
    kms = P.sb("kms", [128, 32, 8], F32); tkm = Trk("kms")
    douts = []

    def load_norm(kind):
        for c in range(32):
            b = c % 2
            P.dma("sp", S.mc[b][:], h1s[c * 128:(c + 1) * 128, :], reads=[th1[c]], writes=[S.tmc[b]])
            P.op("dve", lambda e: e.scalar_tensor_tensor(out=S.AT[:, c, :], in0=S.mc[b][:], scalar=S.vS[:, kind, c:c + 1], in1=S.rstd[:],
                                                         op0=ALU.mult, op1=ALU.mult), reads=[S.tmc[b], S.trstd, S.tv], writes=[S.tAT])

    def epi_out(out_ap, dt, func, km=False):
        def f(j, tb, pb, tpb):
            s_, ts_ = S.stage(dt)
            P.op("act", lambda e: e.activation(out=s_[:], in_=pb[:], func=func), reads=[tpb], writes=[ts_])
            douts.append(ts_)
            P.dma("sp", out_ap[j * 128:(j + 1) * 128, tb * 512:(tb + 1) * 512], s_[:], reads=[ts_])
            if km:
                P.op("dve", lambda e: e.tensor_reduce(out=kms[:, j, tb * 2:(tb + 1) * 2], in_=s_[:].rearrange("p (a b) -> p a b", a=2), axis=AX.X, op=ALU.add),
                     reads=[ts_], writes=[tkm])
        return f
    load_norm(1)
    if stop <= 3:
        return S.finish([])
    S.gemm(w_k, 0, 4096, 32, epi_out(kT, F32, AF.Copy, km=True))
    if stop <= 4:
        return S.finish([])
    S.gemm(w_v, 0, 4096, 32, epi_out(vT, F32, AF.Copy))
    P.op("dve", lambda e: e.tensor_scalar(out=kms[:], in0=kms[:], scalar1=1.0 / 256, scalar2=None, op0=ALU.mult), reads=[tkm], writes=[tkm])
    P.dma("sp", kmT.rearrange("(j p) b -> p j b", p=128), kms[:], reads=[tkm])
    load_norm(2)
    S.gemm(w_qg, 0, 4096, 32, epi_out(qT, F32, AF.Copy))
    S.gemm(w_qg, 4096, 4096, 32, epi_out(gsT, F32, AF.Silu))
    print("B insts", P.n_inst)
    return S.finish(S.tstg + S.tstb + [tkm] + S.tmc)


def build_D():
    S = Stage("D"); P = S.P
    aT = S.din("aT", [4096, TOK]); hT = S.din("hT", [4096, TOK]); vec = S.din("vec", [128, 1, 32])
    w_o = S.din("w_o", [4096, 4096])
    oT = S.dout("oT", [4096, TOK])
    mix = S.dscr("mix", [4096, TOK])
    S.vS = P.sb("vS", [128, 1, 32], F32); S.tv = Trk("vec")
    P.dma("sp", S.vS[:], vec, writes=[S.tv])
    alloc_chunks(S)
    S.alloc_AT()
    to = [Trk(f"o_{j}") for j in range(32)]
    for _ in wo_norm_res(S, aT, hT, w_o, lambda j: S.vS[:, 0, j:j + 1], oT, to, mix):
        pass
    print("D insts", P.n_inst)
    return S.finish(S.tmc + to)


SCALE = 128 ** -0.5
NEG = -1.0e30


def build_C(NH, T=8192, stop=99):
    NB = T // 256
    nc = bass.Bass("TRN2", target_bir_lowering=False)
    qT = nc.dram_tensor("qT", [NH, 128, T], F32, kind="ExternalInput").ap()
    kT = nc.dram_tensor("kT", [NH, 128, T], F32, kind="ExternalInput").ap()
    vv = nc.dram_tensor("v", [NH, T, 128], F32, kind="ExternalInput").ap()
    km = nc.dram_tensor("km", [NH, 128, NB], F32, kind="ExternalInput").ap()
    gs = nc.dram_tensor("gs", [NH, T, 128], F32, kind="ExternalInput").ap()
    hx = nc.dram_tensor("hx", [1, NH], F32, kind="ExternalInput").ap()
    att = nc.dram_tensor("att", [NH, T, 128], F32, kind="ExternalOutput").ap()
    dbg = nc.dram_tensor("dbg", [NH, 128, (T // 256) * 4], F32, kind="ExternalOutput").ap() if stop < 99 else None
    with contextlib.ExitStack() as st:
        P = Prog(nc, st)
        sb, ps = P.sb, P.ps
        tC = Trk("C")
        ones_b = sb("ones_b", [128, 256], BF16)
        maskc = sb("maskc", [128, 2, 256], BF16)
        P.op("pool", lambda e: e.memset(ones_b[:], 1.0), writes=[tC])
        for half in range(2):
            P.op("pool", lambda e: e.affine_select(out=maskc[:, half, :], in_=ones_b[:], pattern=[[1, 256]], compare_op=ALU.is_ge, fill=0.0,
                                                   base=-128 * half, channel_multiplier=-1), reads=[tC], writes=[tC])
        slp = sb("slp", [128, NH], F32); tslp = Trk("slp")
        P.dma("sp", slp[:], hx.partition_broadcast(128), writes=[tslp])
        P.op("act", lambda e: e.activation(out=slp[:], in_=slp[:], func=AF.Exp, scale=-math.log(2.0) / 4.0), reads=[tslp], writes=[tslp])
        Qf = sb("Qf", [128, T], F32); tQf = Trk("Qf")
        Qb = sb("Qb", [128, T], BF16); tQb = Trk("Qb")
        Kb = sb("Kb", [128, T], BF16); tKb = Trk("Kb")
        V1 = sb("V1", [128, T // 128, 130], BF16); tV1 = Trk("V1")
        P.op("pool", lambda e: e.memset(V1[:, :, 128:129], 1.0), writes=[tV1])
        kmS = sb("kmS", [128, NB], F32); tkm = Trk("km")
        G = sb("G", [128, T // 128, 128], F32); tG = Trk("G")
        bias_i = sb("bias_i", [128, NB * 2 * 4], mybir.dt.int32)
        biasq = [sb(f"biasq{i}", [128, NB, 2], F32) for i in range(2)]; tbias = Trk("bias")
        gt = [sb(f"gt{i}", [128, 32], F32) for i in range(2)]; tgt = [Trk("gt0"), Trk("gt1")]
        m8 = [sb(f"m8{i}", [128, 8], F32) for i in range(2)]
        sel = [sb(f"sel{i}", [128, 32], F32) for i in range(2)]; tsel = [Trk("sel0"), Trk("sel1")]
        acc = [sb(f"acc{i}", [128, 129], F32) for i in range(2)]; tacc = [Trk("acc0"), Trk("acc1")]
        rec = [sb(f"rec{i}", [128, 1], F32) for i in range(2)]
        ot = [sb(f"ot{i}", [128, 128], F32) for i in range(2)]; tot = [Trk("ot0"), Trk("ot1")]
        PT = [[[sb(f"PT{r}_{h}_{q}", [128, 128], BF16) for q in range(2)] for h in range(2)] for r in range(3)]
        tPT = [[[Trk(f"PT{r}_{h}_{q}") for q in range(2)] for h in range(2)] for r in range(3)]
        sc = [ps(f"sc{i}", [128, 512]) for i in range(3)]; tsc = [[Trk(f"sc{i}_{k}") for k in range(4)] for i in range(3)]
        po = [ps(f"po{i}", [128, 512]) for i in range(3)]; tpob = [Trk(f"pob{i}") for i in range(3)]
        tscb = [Trk(f"scb{i}") for i in range(3)]
        pgt = ps("pgt", [128, 512]); tpgt = Trk("pgt")
        sci = 0; poi = 0; pti = 0
        for hi in range(NH):
            for q in range(4):
                tsl = slice(q * (T // 4), (q + 1) * (T // 4))
                P.dma("sp", Qf[:, tsl], qT[hi][:, tsl], writes=[tQf])
                P.dma("pool", Kb[:, tsl], kT[hi][:, tsl], writes=[tKb])
                csl = slice(q * (T // 512), (q + 1) * (T // 512))
                P.dma("pool", V1[:, csl, 0:128], vv[hi][tsl, :].rearrange("(c p) d -> p c d", p=128), writes=[tV1])
                P.dma("act", G[:, csl, :], gs[hi][tsl, :].rearrange("(c p) d -> p c d", p=128), writes=[tG])
                P.op("pool", lambda e: e.tensor_copy(out=Qb[:, tsl], in_=Qf[:, tsl]), reads=[tQf], writes=[tQb])
            P.dma("sp", kmS[:], km[hi], writes=[tkm])
            CST = 30.0
            for qs in range(2):
                imax = qs * 128 + 127
                bq = biasq[qs][:].rearrange("p d h -> p (d h)")
                P.op("pool", lambda e: e.iota(bias_i[:, 0:NB * 2], pattern=[[-256, NB], [128, 2], [-256, 1]], base=-imax, channel_multiplier=1),
                     writes=[tbias])
                P.op("pool", lambda e: e.tensor_copy(out=bq, in_=bias_i[:, 0:NB * 2]), reads=[tbias], writes=[tbias])
                P.op("dve", lambda e: e.tensor_scalar(out=bq, in0=bq, scalar1=slp[:, hi:hi + 1], scalar2=None, op0=ALU.mult), reads=[tbias, tslp], writes=[tbias])
                P.op("dve", lambda e: e.tensor_scalar(out=bq, in0=bq, scalar1=CST, scalar2=None, op0=ALU.add), reads=[tbias], writes=[tbias])
            if stop < 99:
                for qs in range(2):
                    P.dma("sp", dbg[hi][:, qs * NB * 2:(qs + 1) * NB * 2], biasq[qs][:].rearrange("p d h -> p (d h)"), reads=[tbias])
            if stop <= 1:
                continue
            for m in range(NB):
                q0 = m * 256
                if m > 0:
                    for qs in range(2):
                        P.op("pe", lambda e: e.matmul(pgt[:, qs * 32:qs * 32 + NB], Qf[:, q0 + qs * 128:q0 + qs * 128 + 128], kmS[:], start=True, stop=True),
                             reads=[tQf, tkm], writes=[tpgt], same_eng_raw=False)
                        P.op("pool", lambda e: e.memset(gt[qs][:], NEG), writes=[tgt[qs]])
                        P.op("dve", lambda e: e.tensor_copy(out=gt[qs][:, 0:m], in_=pgt[:, qs * 32:qs * 32 + m]), reads=[tpgt], writes=[tgt[qs]])
                        P.op("dve", lambda e: e.max(m8[qs][:], gt[qs][:]), reads=[tgt[qs]], writes=[tsel[qs]])
                        P.op("dve", lambda e: e.tensor_scalar(out=sel[qs][:], in0=gt[qs][:], scalar1=m8[qs][:, 2:3], scalar2=None, op0=ALU.is_ge),
                             reads=[tgt[qs], tsel[qs]], writes=[tsel[qs]])
                if stop <= 2:
                    continue
                for n in range(m, -1, -1):
                    d = m - n
                    own = (d == 0)
                    pt_, tpt_ = PT[pti % 3], tPT[pti % 3]; pti += 1
                    s_, ts_ = sc[sci % 3], tscb[sci % 3]; sci += 1
                    tiles = [(half, qs) for half in range(2) for qs in range(2) if not (own and half == 1 and qs == 0)]
                    for (half, qs) in tiles:
                        k0 = n * 256 + half * 128
                        so = (half * 2 + qs) * 128
                        P.op("pe", lambda e: e.matmul(s_[:, so:so + 128], Kb[:, k0:k0 + 128], Qb[:, q0 + qs * 128:q0 + qs * 128 + 128], start=True, stop=True),
                             reads=[tKb, tQb], writes=[ts_], same_eng_raw=False)
                    for (half, qs) in tiles:
                        so = (half * 2 + qs) * 128
                        P.op("act", lambda e: e.activation(out=pt_[half][qs][:], in_=s_[:, so:so + 128], func=AF.Exp,
                                                           bias=biasq[qs][:, d, half:half + 1], scale=SCALE), reads=[ts_, tbias], writes=[tpt_[half][qs]])
                        if own and half == qs:
                            P.op("dve", lambda e: e.tensor_tensor(out=pt_[half][qs][:], in0=pt_[half][qs][:], in1=maskc[:, 0, 0:128], op=ALU.mult),
                                 reads=[tpt_[half][qs], tC], writes=[tpt_[half][qs]])
                    if stop <= 3:
                        continue
                    o_, to_ = po[poi % 3], tpob[poi % 3]; poi += 1
                    for qs in range(2):
                        oo = qs * 256
                        halves = [0] if (own and qs == 0) else [0, 1]
                        for ih, half in enumerate(halves):
                            P.op("pe", lambda e: e.matmul(o_[:, oo:oo + 129], pt_[half][qs][:], V1[:, n * 2 + half, 0:129],
                                                          start=(ih == 0), stop=(ih == len(halves) - 1)),
                                 reads=[tpt_[half][qs], tV1], writes=[to_], same_eng_raw=False)
                    for qs in range(2):
                        oo = qs * 256
                        if own:
                            P.op("dve", lambda e: e.tensor_copy(out=acc[qs][:], in_=o_[:, oo:oo + 129]), reads=[to_], writes=[tacc[qs]])
                        else:
                            P.op("dve", lambda e: e.scalar_tensor_tensor(out=acc[qs][:], in0=o_[:, oo:oo + 129], scalar=sel[qs][:, n:n + 1], in1=acc[qs][:],
                                                                         op0=ALU.mult, op1=ALU.add), reads=[to_, tsel[qs], tacc[qs]], writes=[tacc[qs]])
                if stop <= 3:
                    continue
                for qs in range(2):
                    P.op("dve", lambda e: e.reciprocal(out=rec[qs][:], in_=acc[qs][:, 128:129]), reads=[tacc[qs]], writes=[tacc[qs]])
                    P.op("dve", lambda e: e.scalar_tensor_tensor(out=ot[qs][:], in0=acc[qs][:, 0:128], scalar=rec[qs][:, 0:1], in1=G[:, m * 2 + qs, :],
                                                                 op0=ALU.mult, op1=ALU.mult), reads=[tacc[qs], tG], writes=[tot[qs]])
                    P.dma("sp", att[hi][q0 + qs * 128:q0 + qs * 128 + 128, :], ot[qs][:], reads=[tot[qs]])
        P.wait_everything("sp")
        print("C insts", P.n_inst)
    return nc


from concourse.bass_utils import run_bass_kernel_spmd
import ml_dtypes as _mld


def _pc(v):
    return np.ascontiguousarray(v.reshape(32, 128).T)


def _run(nc, maps):
    return run_bass_kernel_spmd(nc, maps, core_ids=list(range(8))).results


def _tokT(arr, c):
    b, s = c // 4, (c % 4) * TOK
    return np.ascontiguousarray(arr[b, s:s + TOK].T)


def _gather_T(res, name, dtype=np.float32):
    out = np.empty((2, 8192, 4096), dtype)
    for c in range(8):
        b, s = c // 4, (c % 4) * TOK
        out[b, s:s + TOK] = np.asarray(res[c][name]).T
    return out


def kernel(**inp):
    inp = {k: np.asarray(v) for k, v in inp.items()}
    x = inp["x"]
    f32 = np.float32
    vecA = np.ascontiguousarray(np.stack([_pc(inp["a_pre_g"][0])] + [_pc(inp["a_mu"][0, i]) for i in range(6)]
                                         + [_pc(inp["a_w0"][0]), _pc(inp["a_a0"][0])], axis=1)).astype(f32)
    maps = []
    for c in range(8):
        b, s = c // 4, (c % 4) * TOK
        xp = np.zeros((128, 32), f32) if s == 0 else _pc(x[b, s - 1])
        maps.append({"xT": _tokT(x, c), "xp": xp, "vec": vecA, "w_in": inp["a_w_in"][0], "w1": inp["a_w1"][0], "w2": inp["a_w2"][0],
                     "a1": inp["a_a1"][0], "a2": inp["a_a2"][0]})
    res = _run(build_A1(), maps)
    r = _gather_T(res, "rT"); k = _gather_T(res, "kT"); v = _gather_T(res, "vT")
    gsA = _gather_T(res, "gsT"); sg = _gather_T(res, "sgT"); a = _gather_T(res, "aT")
    del res, maps
    NCH = 64
    maps = []
    for c in range(8):
        b, g = c // 4, c % 4
        cs = slice(g * 1024, (g + 1) * 1024)

        def fmaj(X):
            return X[b][:, cs].reshape(NCH, 128, 8, 128).transpose(0, 3, 2, 1)
        fm = np.ascontiguousarray(np.stack([fmaj(r), fmaj(k), fmaj(a)], axis=2)).astype(f32)
        tm = np.ascontiguousarray(np.stack([v[b][:, cs].reshape(NCH, 128, 1024), gsA[b][:, cs].reshape(NCH, 128, 1024),
                                            sg[b][:, cs].reshape(NCH, 128, 1024)], axis=2)).astype(f32)

        def cvec(z):
            return z[cs].reshape(8, 128).T
        cv = np.ascontiguousarray(np.stack([cvec(inp["a_k_k"][0]), cvec(inp["a_k_a"][0]), cvec(inp["a_r_k"][0].reshape(-1))], axis=1)).astype(f32)
        ln = np.ascontiguousarray(np.stack([inp["a_lnx_w"][0][cs], inp["a_lnx_b"][0][cs]])).astype(f32)
        maps.append({"fm": fm, "tm": tm, "cv": cv, "ln": ln})
    res = _run(build_scan(NCH), maps)
    y = np.empty((2, 8192, 4096), f32)
    for c in range(8):
        b, g = c // 4, c % 4
        y[b][:, g * 1024:(g + 1) * 1024] = np.asarray(res[c]["y"]).reshape(8192, 1024)
    del res, maps, r, k, v, gsA, sg, a
    vecB = np.ascontiguousarray(np.stack([_pc(inp["a_post_g"][0]), _pc(inp["kv_norm_g"]), _pc(inp["b_pre_g"][0])], axis=1)).astype(f32)
    maps = [{"yT": _tokT(y, c), "xT": _tokT(x, c), "vec": vecB, "w_o": inp["a_w_o"][0], "w_k": inp["w_k"], "w_v": inp["w_v"],
             "w_qg": inp["b_w_qg"][0]} for c in range(8)]
    res = _run(build_B(), maps)
    h1 = _gather_T(res, "h1T"); K = _gather_T(res, "kT"); V = _gather_T(res, "vT")
    Q = _gather_T(res, "qT"); GS = _gather_T(res, "gsT")
    KM = np.empty((2, 32, 4096), f32)
    for c in range(8):
        b, s = c // 4, (c % 4) * 8
        KM[b, s:s + 8] = np.asarray(res[c]["kmT"]).T
    del res, maps, y
    maps = []
    hsets = []
    for c in range(8):
        b, g = c // 4, c % 4
        heads = [g + 4 * i for i in range(8)]
        hsets.append(heads)
        hs = [slice(h * 128, (h + 1) * 128) for h in heads]
        maps.append({"qT": np.ascontiguousarray(np.stack([Q[b][:, s_].T for s_ in hs])),
                     "kT": np.ascontiguousarray(np.stack([K[b][:, s_].T for s_ in hs])),
                     "v": np.ascontiguousarray(np.stack([V[b][:, s_] for s_ in hs])),
                     "km": np.ascontiguousarray(np.stack([KM[b][:, s_].T for s_ in hs])),
                     "gs": np.ascontiguousarray(np.stack([GS[b][:, s_] for s_ in hs])),
                     "hx": np.array([[h + 1 for h in heads]], np.float32)})
    att = np.empty((2, 8192, 4096), f32)
    res = _run(build_C(8), maps)
    for c in range(8):
        b = c // 4
        for i, h in enumerate(hsets[c]):
            att[b][:, h * 128:(h + 1) * 128] = np.asarray(res[c]["att"])[i]
    del res, maps, K, V, Q, GS
    vecD = np.ascontiguousarray(_pc(inp["b_post_g"][0])[:, None, :]).astype(f32)
    maps = [{"aT": _tokT(att, c), "hT": _tokT(h1, c), "vec": vecD, "w_o": inp["b_w_o"][0]} for c in range(8)]
    res = _run(build_D(), maps)
    return _gather_T(res, "oT")
```

```python
import contextlib, math
import numpy as np
import concourse.bass as bass
import concourse.mybir as mybir

F32 = mybir.dt.float32
BF16 = mybir.dt.bfloat16
ALU = mybir.AluOpType
AF = mybir.ActivationFunctionType
AX = mybir.AxisListType

SEM_ROT = 30000
N_DMA_SEMS = 40


class Trk:
    __slots__ = ("w", "r", "name")

    ALL = []

    def __init__(self, name=""):
        self.w = []
        self.r = []
        self.name = name
        Trk.ALL.append(self)


class Prog:
    def __init__(self, nc, stack):
        Trk.ALL.clear()
        self.nc = nc
        self.stack = stack
        self.E = {"pe": nc.tensor, "act": nc.scalar, "dve": nc.vector, "pool": nc.gpsimd, "sp": nc.sync}
        self.sems = {}
        self.cur = {}
        self.cnt = {}
        self.seen = {e: {} for e in self.E}
        self.nsem = 0
        for e in self.E:
            self._new_eng_sem(e)
        self.dma_keys = []
        for i in range(N_DMA_SEMS):
            k = f"dma{i}"
            self.sems[k] = stack.enter_context(nc.semaphore(k))
            self.cnt[k] = 0
            self.dma_keys.append(k)
        self.dma_rr = 0
        self.n_inst = 0

    def _new_eng_sem(self, e):
        k = f"s_{e}_{self.nsem}"
        self.nsem += 1
        self.sems[k] = self.stack.enter_context(self.nc.semaphore(k))
        self.cnt[k] = 0
        self.cur[e] = k

    def _wait(self, e, tok):
        k, v = tok
        if self.seen[e].get(k, 0) >= v:
            return
        self.seen[e][k] = v
        self.E[e].wait_ge(self.sems[k], v)

    def _deps(self, e, reads, writes, same_eng_raw=True):
        own = self.cur[e]
        for t in reads:
            for tok in t.w:
                if tok[0] == own and not same_eng_raw:
                    continue
                self._wait(e, tok)
        for t in writes:
            for tok in t.w:
                if tok[0] == own and not same_eng_raw:
                    continue
                self._wait(e, tok)
            for tok in t.r:
                if tok[0] == own:
                    continue
                self._wait(e, tok)

    def _commit(self, tok, reads, writes):
        for t in reads:
            t.r = [x for x in t.r if x[0] != tok[0]] + [tok]
        for t in writes:
            t.w = [tok]
            t.r = []

    def op(self, e, fn, reads=(), writes=(), same_eng_raw=True):
        if self.cnt[self.cur[e]] >= SEM_ROT:
            self._new_eng_sem(e)
        self._deps(e, reads, writes, same_eng_raw)
        k = self.cur[e]
        inst = fn(self.E[e])
        self.cnt[k] += 1
        inst.then_inc(self.sems[k], 1)
        tok = (k, self.cnt[k])
        self._commit(tok, reads, writes)
        self.n_inst += 1
        return tok

    def dma(self, q, out, in_, reads=(), writes=(), **kw):
        k = self.dma_keys[self.dma_rr % len(self.dma_keys)]
        self.dma_rr += 1
        if self.cnt[k] > 0:
            self._wait(q, (k, self.cnt[k]))
        for t in reads:
            for tok in t.w:
                self._wait(q, tok)
        for t in writes:
            for tok in t.w + t.r:
                self._wait(q, tok)
        inst = self.E[q].dma_start(out=out, in_=in_, **kw)
        self.cnt[k] += 16
        inst.then_inc(self.sems[k], 16)
        tok = (k, self.cnt[k])
        self._commit(tok, reads, writes)
        self.n_inst += 1
        return tok

    def wait_everything(self, e="sp"):
        self.wait_all(e, Trk.ALL)
        Trk.ALL.clear()

    def wait_all(self, e, trks):
        for t in trks:
            for tok in t.w + t.r:
                self._wait(e, tok)

    def sb(self, name, shape, dt):
        return self.stack.enter_context(self.nc.sbuf_tensor(name, list(shape), dt))

    def ps(self, name, shape, dt=F32):
        return self.stack.enter_context(self.nc.psum_tensor(name, list(shape), dt))


C_DEC = 0.6065306597126334
GN_EPS = 64e-5


def build_scan(NCH, out_dt=F32):
    nc = bass.Bass("TRN2", target_bir_lowering=False)
    fm = nc.dram_tensor("fm", [NCH, 128, 3, 8, 128], F32, kind="ExternalInput").ap()
    tm = nc.dram_tensor("tm", [NCH, 128, 3, 1024], F32, kind="ExternalInput").ap()
    cv = nc.dram_tensor("cv", [128, 3, 8], F32, kind="ExternalInput").ap()
    ln = nc.dram_tensor("ln", [2, 1024], F32, kind="ExternalInput").ap()
    yo = nc.dram_tensor("y", [NCH, 128, 1024], out_dt, kind="ExternalOutput").ap()
    with contextlib.ExitStack() as st:
        P = Prog(nc, st)
        sb, ps = P.sb, P.ps
        tC = Trk("const")
        ones_f = sb("ones_f", [128, 256], F32)
        negc = sb("negc", [128, 128], F32)
        ident_f = sb("ident_f", [128, 128], F32)
        ident_b = sb("ident_b", [128, 128], BF16)
        idpair = sb("idpair", [128, 64], F32)
        maskl = sb("maskl", [128, 128], F32)
        masku4 = sb("masku4", [128, 512], F32)
        tri2 = sb("tri2", [128, 256], F32)
        blk1 = sb("blk1", [128, 128], F32)
        onesblk = sb("onesblk", [128, 2], BF16)
        cvS = sb("cvS", [128, 3, 8], F32)
        omk = sb("omk", [128, 8], F32)
        lnS = sb("lnS", [128, 2, 1024], F32)
        G = P.E["pool"]
        P.op("pool", lambda e: e.memset(ones_f[:], 1.0), writes=[tC])
        P.op("pool", lambda e: e.memset(negc[:], -C_DEC), writes=[tC])
        P.op("pool", lambda e: e.affine_select(out=ident_f[:], in_=ones_f[:, 0:128], pattern=[[-1, 128]],
                                               compare_op=ALU.is_equal, fill=0.0, base=0, channel_multiplier=1), reads=[tC], writes=[tC])
        P.op("pool", lambda e: e.tensor_copy(out=ident_b[:], in_=ident_f[:]), reads=[tC], writes=[tC])
        P.op("pool", lambda e: e.tensor_copy(out=idpair[0:64, :], in_=ident_f[0:64, 0:64]), reads=[tC], writes=[tC])
        P.op("pool", lambda e: e.tensor_copy(out=idpair[64:128, :], in_=ident_f[64:128, 64:128]), reads=[tC], writes=[tC])
        P.op("pool", lambda e: e.affine_select(out=maskl[:], in_=ones_f[:, 0:128], pattern=[[-1, 128]],
                                               compare_op=ALU.is_gt, fill=0.0, base=0, channel_multiplier=1), reads=[tC], writes=[tC])
        for q in range(2):
            P.op("pool", lambda e: e.affine_select(out=masku4[:, q * 256:q * 256 + 128], in_=ones_f[:, 0:128], pattern=[[1, 128]],
                                                   compare_op=ALU.is_gt, fill=0.0, base=0, channel_multiplier=-1), reads=[tC], writes=[tC])
            P.op("pool", lambda e: e.affine_select(out=masku4[:, q * 256 + 128:q * 256 + 256], in_=ones_f[:, 0:128], pattern=[[1, 128]],
                                                   compare_op=ALU.is_ge, fill=0.0, base=0, channel_multiplier=-1), reads=[tC], writes=[tC])
        P.op("pool", lambda e: e.affine_select(out=tri2[:, 0:128], in_=negc[:], pattern=[[1, 128]],
                                               compare_op=ALU.is_ge, fill=0.0, base=0, channel_multiplier=-1), reads=[tC], writes=[tC])
        P.op("pool", lambda e: e.affine_select(out=tri2[:, 128:256], in_=negc[:], pattern=[[1, 128]],
                                               compare_op=ALU.is_gt, fill=0.0, base=0, channel_multiplier=-1), reads=[tC], writes=[tC])
        P.op("pool", lambda e: e.memset(blk1[:], 0.0), writes=[tC])
        P.op("pool", lambda e: e.memset(blk1[0:64, 0:64], 1.0), writes=[tC])
        P.op("pool", lambda e: e.memset(blk1[64:128, 64:128], 1.0), writes=[tC])
        P.op("pool", lambda e: e.memset(onesblk[:], 0.0), writes=[tC])
        P.op("pool", lambda e: e.memset(onesblk[0:64, 0:1], 1.0), writes=[tC])
        P.op("pool", lambda e: e.memset(onesblk[64:128, 1:2], 1.0), writes=[tC])
        P.dma("sp", cvS[:], cv, writes=[tC])
        P.dma("sp", lnS[:], ln.partition_broadcast(128), writes=[tC])
        P.op("dve", lambda e: e.tensor_scalar(out=omk[:], in0=cvS[:, 1, :], scalar1=-1.0, scalar2=1.0, op0=ALU.mult, op1=ALU.add),
             reads=[tC], writes=[tC])

        def bc8(ap2):
            return ap2.unsqueeze(2).to_broadcast([128, 8, 128])

        S = [sb(f"S{i}", [128, 8, 64], F32) for i in range(2)]
        tS = [Trk("S0"), Trk("S1")]
        P.op("pool", lambda e: e.memset(S[0][:], 0.0), writes=[tS[0]])
        Z = sb("Z", [128, 16, 2, 128], BF16); tZ = [Trk(f"Z{h}") for h in range(16)]
        Vz = sb("Vz", [128, 16, 128], BF16); tVz = Trk("Vz")
        PTbd = sb("PTbd", [128, 8, 128], F32); tPT = [Trk(f"PT{c}") for c in range(8)]
        P.op("pool", lambda e: e.memset(Z[:], 0.0), writes=tZ)
        P.op("pool", lambda e: e.memset(Vz[:], 0.0), writes=[tVz])
        P.op("pool", lambda e: e.memset(PTbd[:], 0.0), writes=tPT)
        Lv = [[sb(f"Lv{p}_{j}", [128, 384], BF16) for j in range(7)] for p in range(2)]
        tLv = [[Trk(f"Lv{p}_{j}") for j in range(7)] for p in range(2)]
        for p in range(2):
            P.op("pool", lambda e: e.tensor_copy(out=Lv[p][0][:, 128:256], in_=ident_f[:]), reads=[tC], writes=[tLv[p][0]])
        LR = [sb(f"LR{p}", [128, 384], BF16) for p in range(2)]; tLR = [Trk("LR0"), Trk("LR1")]
        TT = [sb(f"TT{p}", [128, 128], BF16) for p in range(2)]; tTT = [Trk("TT0"), Trk("TT1")]
        AW = sb("AW", [128, 16, 128], BF16); tAW = [Trk(f"AW{h}") for h in range(16)]
        fmS = [sb(f"fmS{i}", [128, 3, 8, 128], F32) for i in range(2)]; tfm = [Trk("fm0"), Trk("fm1")]
        tmS = [sb(f"tmS{i}", [128, 3, 1024], F32) for i in range(2)]; ttm = [Trk("tm0"), Trk("tm1")]
        E12 = sb("E12", [128, 8, 256], F32); tE12 = Trk("E12")
        E3 = sb("E3", [128, 8, 128], F32); tE3 = Trk("E3")
        E4 = sb("E4", [128, 8, 128], F32); tE4 = Trk("E4")
        Et = sb("Et", [128, 8, 128], F32); tEt = Trk("Et")
        mpos = sb("mpos", [128, 8], F32); negm = sb("negm", [128, 8], F32); cC = sb("cC", [128, 8], F32)
        em = sb("em", [128, 8], F32); gC = sb("gC", [128, 8], F32); tsm = Trk("small")
        kkraw = sb("kkraw", [128, 8, 128], F32); tkkraw = Trk()
        sq = sb("sq", [128, 8, 128], F32); tsq = Trk()
        rn = sb("rn", [128, 8, 128], F32); trn = Trk()
        kk = sb("kk", [128, 8, 128], F32); tkk = Trk()
        uu = sb("uu", [128, 8, 128], F32); tuu = Trk()
        kmod = sb("kmod", [128, 8, 128], F32); tkmod = Trk()
        beta = sb("beta", [128, 8, 128], F32); tbeta = Trk()
        RA = sb("RA", [128, 8, 256], BF16); tRA = Trk()
        BsT = sb("BsT", [128, 8, 128], BF16); tBsT = Trk()
        KsT = sb("KsT", [128, 8, 128], BF16); tKsT = Trk()
        BpF = sb("BpF", [128, 8, 128], BF16); tBpF = Trk()
        KpF = sb("KpF", [128, 8, 128], BF16); tKpF = Trk()
        RtT = sb("RtT", [128, 8, 128], F32); tRtT = Trk()
        prodT = sb("prodT", [128, 8, 128], BF16); tprod = Trk()
        BpT = sb("BpT", [128, 1024], BF16); tBpT = Trk()
        KpT = sb("KpT", [128, 1024], BF16); tKpT = Trk()
        DG = sb("DG", [128, 8, 64], F32); tDG = Trk()
        Qall = sb("Qall", [128, 8, 64], F32); tQ = Trk()
        RhT = sb("RhT", [128, 8, 128], F32); tRh = [Trk(f"Rh{c}") for c in range(8)]
        bon = sb("bon", [128, 16], F32); tbon = Trk()
        ysb = sb("ysb", [128, 16, 64], F32); tysb = Trk()
        ysq = sb("ysq", [128, 16, 64], F32); tysq = Trk()
        st1 = sb("st1", [128, 4, 16], F32); tst = Trk()
        yout = sb("yout", [128, 1024], out_dt); tyo = Trk()
        pc = ps("pc", [128, 2, 512]); tpc = [Trk("pc0"), Trk("pc1")]
        pt = ps("pt", [128, 1024], BF16); tpt = Trk("pt")
        ph = ps("ph", [128, 512]); tph = Trk("ph")
        pi = [ps(f"pi{i}", [128, 512]) for i in range(2)]; tpi = [Trk("pi0"), Trk("pi1")]
        pq = ps("pq", [128, 512]); tpq = Trk("pq")
        pS = ps("pS", [128, 8, 64]); tpS = Trk("pS")

        def load(n):
            b = n % 2
            P.dma("sp", fmS[b][:], fm[n], writes=[tfm[b]])
            P.dma("act", tmS[b][:], tm[n], writes=[ttm[b]])

        load(0)
        for n in range(NCH):
            b = n % 2
            if n + 1 < NCH:
                load(n + 1)
            R_ = fmS[b][:, 0]; K_ = fmS[b][:, 1]; A_ = fmS[b][:, 2]
            V_ = tmS[b][:, 0, :]; GS_ = tmS[b][:, 1, :]; SG_ = tmS[b][:, 2, :]
            Sp, Sn = S[b], S[1 - b]; tSp, tSn = tS[b], tS[1 - b]
            for half in range(2):
                for j in range(4):
                    cg = half * 4 + j
                    P.op("pe", lambda e: e.matmul(pc[:, j // 2, (j % 2) * 256:(j % 2) * 256 + 256], SG_[:, cg * 128:(cg + 1) * 128], tri2[:],
                                                  start=True, stop=True),
                         reads=[ttm[b], tC], writes=[tpc[j // 2]], same_eng_raw=False)
                pcv = pc[:].rearrange("p a (b c) -> p (a b) c", b=2)
                sl = slice(half * 4, half * 4 + 4)
                P.op("dve", lambda e: e.tensor_copy(out=mpos[:, sl], in_=pcv[:, :, 63]), reads=tpc, writes=[tsm])
                P.op("dve", lambda e: e.tensor_scalar(out=negm[:, sl], in0=pcv[:, :, 63], scalar1=-1.0, scalar2=None, op0=ALU.mult), reads=tpc, writes=[tsm])
                P.op("dve", lambda e: e.tensor_copy(out=cC[:, sl], in_=pcv[:, :, 127]), reads=tpc, writes=[tsm])
                for j in range(4):
                    cg = half * 4 + j
                    P.op("act", lambda e: e.activation(out=E12[:, cg, :], in_=pcv[:, j, :], func=AF.Exp, bias=negm[:, cg:cg + 1], scale=1.0),
                         reads=tpc + [tsm], writes=[tE12])
                    P.op("act", lambda e: e.activation(out=E3[:, cg, :], in_=pcv[:, j, 0:128], func=AF.Exp, bias=mpos[:, cg:cg + 1], scale=-1.0),
                         reads=tpc + [tsm], writes=[tE3])
                    P.op("act", lambda e: e.activation(out=E4[:, cg, :], in_=pcv[:, j, 0:128], func=AF.Exp, bias=cC[:, cg:cg + 1], scale=-1.0),
                         reads=tpc + [tsm], writes=[tE4])
                    P.op("act", lambda e: e.activation(out=Et[:, cg, :], in_=pcv[:, j, 0:128], func=AF.Exp),
                         reads=tpc + [tsm], writes=[tEt])
            P.op("act", lambda e: e.activation(out=em[:], in_=mpos[:], func=AF.Exp), reads=[tsm], writes=[tsm])
            P.op("act", lambda e: e.activation(out=gC[:], in_=cC[:], func=AF.Exp), reads=[tsm], writes=[tsm])
            P.op("dve", lambda e: e.tensor_tensor(out=DG[:], in0=idpair[:].unsqueeze(1).to_broadcast([128, 8, 64]),
                                                  in1=gC[:].unsqueeze(2).to_broadcast([128, 8, 64]), op=ALU.mult), reads=[tsm, tC], writes=[tDG])
            P.op("pool", lambda e: e.tensor_tensor(out=kkraw[:], in0=K_, in1=bc8(cvS[:, 0, :]), op=ALU.mult), reads=[tfm[b], tC], writes=[tkkraw])
            P.op("pool", lambda e: e.tensor_tensor(out=sq[:], in0=kkraw[:], in1=kkraw[:], op=ALU.mult), reads=[tkkraw], writes=[tsq])
            for cg in range(8):
                P.op("pe", lambda e: e.matmul(pc[:, cg // 4, (cg % 4) * 128:(cg % 4) * 128 + 128], blk1[:], sq[:, cg, :], start=True, stop=True),
                     reads=[tsq, tC], writes=[tpc[cg // 4]], same_eng_raw=False)
            P.op("dve", lambda e: e.tensor_scalar(out=rn[:], in0=pc[:].rearrange("p a (b c) -> p (a b) c", b=4), scalar1=1e-24, scalar2=None,
                                                  op0=ALU.max), reads=tpc, writes=[trn])
            P.op("act", lambda e: e.activation(out=rn[:], in_=rn[:], func=AF.Sqrt), reads=[trn], writes=[trn])
            P.op("dve", lambda e: e.reciprocal(out=rn[:], in_=rn[:]), reads=[trn], writes=[trn])
            P.op("dve", lambda e: e.tensor_tensor(out=kk[:], in0=kkraw[:], in1=rn[:], op=ALU.mult), reads=[tkkraw, trn], writes=[tkk])
            P.op("pool", lambda e: e.tensor_tensor(out=uu[:], in0=A_, in1=bc8(cvS[:, 1, :]), op=ALU.mult), reads=[tfm[b], tC], writes=[tuu])
            P.op("pool", lambda e: e.tensor_tensor(out=uu[:], in0=uu[:], in1=bc8(omk[:]), op=ALU.add), reads=[tuu, tC], writes=[tuu])
            P.op("pool", lambda e: e.tensor_tensor(out=kmod[:], in0=K_, in1=uu[:], op=ALU.mult), reads=[tfm[b], tuu], writes=[tkmod])
            P.op("pool", lambda e: e.tensor_tensor(out=beta[:], in0=kk[:], in1=A_, op=ALU.mult), reads=[tkk, tfm[b]], writes=[tbeta])
            P.op("dve", lambda e: e.scalar_tensor_tensor(out=RA[:, :, 0:128], in0=kk[:], scalar=-1.0, in1=E12[:, :, 128:256], op0=ALU.mult, op1=ALU.mult),
                 reads=[tkk, tE12], writes=[tRA])
            P.op("pool", lambda e: e.tensor_tensor(out=RA[:, :, 128:256], in0=R_, in1=E12[:, :, 0:128], op=ALU.mult), reads=[tfm[b], tE12], writes=[tRA])
            P.op("dve", lambda e: e.tensor_tensor(out=BsT[:], in0=beta[:], in1=E3[:], op=ALU.mult), reads=[tbeta, tE3], writes=[tBsT])
            P.op("pool", lambda e: e.tensor_tensor(out=KsT[:], in0=kmod[:], in1=E3[:], op=ALU.mult), reads=[tkmod, tE3], writes=[tKsT])
            P.op("dve", lambda e: e.tensor_tensor(out=BpF[:], in0=beta[:], in1=E4[:], op=ALU.mult), reads=[tbeta, tE4], writes=[tBpF])
            P.op("pool", lambda e: e.tensor_tensor(out=KpF[:], in0=kmod[:], in1=E4[:], op=ALU.mult), reads=[tkmod, tE4], writes=[tKpF])
            P.op("pool", lambda e: e.tensor_tensor(out=RtT[:], in0=R_, in1=Et[:], op=ALU.mult), reads=[tfm[b], tEt], writes=[tRtT])
            P.op("pool", lambda e: e.tensor_tensor(out=uu[:], in0=R_, in1=kmod[:], op=ALU.mult), reads=[tfm[b], tkmod], writes=[tuu])
            P.op("pool", lambda e: e.tensor_tensor(out=prodT[:], in0=uu[:], in1=bc8(cvS[:, 2, :]), op=ALU.mult), reads=[tuu, tC], writes=[tprod])
            V4 = V_.rearrange("p (c h v) -> p c h v", h=2, v=64)
            Vz4 = Vz[:].rearrange("p (c h) x -> p c h x", h=2)
            P.op("act", lambda e: e.copy(out=Vz4[:, :, 0, 0:64], in_=V4[:, :, 0, :]), reads=[ttm[b]], writes=[tVz])
            P.op("act", lambda e: e.copy(out=Vz4[:, :, 1, 64:128], in_=V4[:, :, 1, :]), reads=[ttm[b]], writes=[tVz])
            for cg in range(8):
                P.op("pe", lambda e: e.matmul(pq[:, 384 + 2 * cg:386 + 2 * cg], prodT[:, cg, :], onesblk[:], start=True, stop=True),
                     reads=[tprod, tC], writes=[tpq], same_eng_raw=False)
            P.op("dve", lambda e: e.tensor_copy(out=bon[:], in_=pq[:, 384:400]), reads=[tpq], writes=[tbon])
            for kind in range(3):
                src_t, trk = [(RA, tRA), (BpF, tBpF), (KpF, tKpF)][kind]
                for cg in range(8):
                    P.op("pe", lambda e: e.transpose(pt[:, cg * 128:(cg + 1) * 128], src_t[:, cg, 0:128], ident_b[:]),
                         reads=[trk, tC], writes=[tpt], same_eng_raw=False)
                if kind == 0:
                    P.op("dve", lambda e: e.tensor_copy(out=AW[:, :, 0:64], in_=pt[:].rearrange("p (h x) -> p h x", x=64)), reads=[tpt], writes=tAW)
                elif kind == 1:
                    P.op("act", lambda e: e.copy(out=BpT[:], in_=pt[:]), reads=[tpt], writes=[tBpT])
                else:
                    P.op("dve", lambda e: e.tensor_copy(out=KpT[:], in_=pt[:]), reads=[tpt], writes=[tKpT])
            for cg in range(8):
                def head_steps(cg, hh):
                    h = 2 * cg + hh
                    hs = slice(hh * 64, hh * 64 + 64)
                    L = Lv[hh]; tL = tLv[hh]
                    pp_, tpp = pi[hh], tpi[hh]
                    steps = []

                    def s_m():
                        P.op("pe", lambda e: e.matmul(ph[:, 0:256], BsT[hs, cg, :], RA[hs, cg, :], start=True, stop=True),
                             reads=[tBsT, tRA], writes=[tph], same_eng_raw=False)
                        P.op("pe", lambda e: e.matmul(ph[:, 256:512], KsT[hs, cg, :], RA[hs, cg, :], start=True, stop=True),
                             reads=[tKsT, tRA], writes=[tph], same_eng_raw=False)
                        P.op("pe", lambda e: e.matmul(pp_[:, 384:512], RA[hs, cg, 0:128], BsT[hs, cg, :], start=True, stop=True),
                             reads=[tBsT, tRA], writes=[tpp], same_eng_raw=False)
                        P.op("dve", lambda e: e.tensor_tensor(out=L[0][:, 0:128], in0=ph[:, 0:128], in1=masku4[:, 0:128], op=ALU.mult),
                             reads=[tph, tC], writes=[tL[0]])
                        P.op("dve", lambda e: e.tensor_tensor(out=LR[hh][:], in0=ph[:, 128:512], in1=masku4[:, 128:512], op=ALU.mult),
                             reads=[tph, tC], writes=[tLR[hh]])
                        P.op("dve", lambda e: e.tensor_tensor(out=L[0][:, 256:384], in0=pp_[:, 384:512], in1=maskl[:], op=ALU.mult),
                             reads=[tpp, tC], writes=[tL[0]])
                    steps.append(s_m)

                    def mk_level(j):
                        def s_l():
                            s_, d_ = L[j], L[j + 1]
                            P.op("pe", lambda e: e.matmul(pp_[:, 0:256], s_[:, 256:384], s_[:, 0:256], start=True, stop=False),
                                 reads=[tL[j]], writes=[tpp], same_eng_raw=False)
                            P.op("pe", lambda e: e.matmul(pp_[:, 128:256], ident_b[:], s_[:, 128:256], start=False, stop=True),
                                 reads=[tL[j], tC], writes=[tpp], same_eng_raw=False)
                            P.op("pe", lambda e: e.matmul(pp_[:, 256:384], s_[:, 0:128], s_[:, 256:384], start=True, stop=True),
                                 reads=[tL[j]], writes=[tpp], same_eng_raw=False)
                            if hh == 0:
                                P.op("act", lambda e: e.copy(out=d_[:], in_=pp_[:, 0:384]), reads=[tpp], writes=[tL[j + 1]])
                            else:
                                P.op("dve", lambda e: e.tensor_copy(out=d_[:], in_=pp_[:, 0:384]), reads=[tpp], writes=[tL[j + 1]])
                        return s_l
                    for j in range(6):
                        steps.append(mk_level(j))

                    def s_fin():
                        P.op("pe", lambda e: e.matmul(pp_[:, 0:128], L[6][:, 256:384], L[6][:, 128:256], start=True, stop=False),
                             reads=[tL[6]], writes=[tpp], same_eng_raw=False)
                        P.op("pe", lambda e: e.matmul(pp_[:, 0:128], ident_b[:], L[6][:, 128:256], start=False, stop=True),
                             reads=[tL[6], tC], writes=[tpp], same_eng_raw=False)
                        P.op("act", lambda e: e.copy(out=TT[hh][:], in_=pp_[:, 0:128]), reads=[tpp], writes=[tTT[hh]])
                    steps.append(s_fin)

                    def s_w2():
                        P.op("pe", lambda e: e.matmul(pp_[:, 0:64], LR[hh][:, 128:256], Vz[:, h, hs], start=True, stop=True),
                             reads=[tLR[hh], tVz], writes=[tpp], same_eng_raw=False)
                        P.op("dve", lambda e: e.tensor_copy(out=AW[:, h, 64:128], in_=pp_[:, 0:64]), reads=[tpp], writes=[tAW[h]])
                    steps.append(s_w2)

                    def s_au():
                        P.op("pe", lambda e: e.matmul(pp_[:, 128:256], TT[hh][:], AW[:, h, :], start=True, stop=True),
                             reads=[tTT[hh], tAW[h]], writes=[tpp], same_eng_raw=False)
                        P.op("act", lambda e: e.copy(out=Z[:, h, :, hs], in_=pp_[:, 128:256].rearrange("p (a b) -> p a b", a=2)),
                             reads=[tpp], writes=[tZ[h]])
                    steps.append(s_au)
                    return steps
                st0, st1_ = head_steps(cg, 0), head_steps(cg, 1)
                for fa, fb in zip(st0, st1_):
                    fa()
                    fb()
                h0, h1 = 2 * cg, 2 * cg + 1
                csl = slice(cg * 128, cg * 128 + 128)
                P.op("pe", lambda e: e.matmul(pq[:, 0:128], Z[:, h0, 0, :], LR[0][:, 0:128], start=True, stop=False),
                     reads=[tZ[h0], tLR[0]], writes=[tpq], same_eng_raw=False)
                P.op("pe", lambda e: e.matmul(pq[:, 0:128], Z[:, h1, 0, :], LR[1][:, 0:128], start=False, stop=True),
                     reads=[tZ[h1], tLR[1]], writes=[tpq], same_eng_raw=False)
                P.op("dve", lambda e: e.scalar_tensor_tensor(out=RhT[:, cg, :], in0=pq[:, 0:128], scalar=em[:, cg:cg + 1], in1=RtT[:, cg, :],
                                                             op0=ALU.mult, op1=ALU.add), reads=[tpq, tsm, tRtT], writes=[tRh[cg]])
                P.op("pe", lambda e: e.matmul(pq[:, 128:256], Z[:, h0, 0, :], BpT[:, csl], start=True, stop=False),
                     reads=[tZ[h0], tBpT], writes=[tpq], same_eng_raw=False)
                P.op("pe", lambda e: e.matmul(pq[:, 128:256], Z[:, h1, 0, :], BpT[:, csl], start=False, stop=True),
                     reads=[tZ[h1], tBpT], writes=[tpq], same_eng_raw=False)
                for hh in range(2):
                    hs = slice(hh * 64, hh * 64 + 64)
                    P.op("dve", lambda e: e.scalar_tensor_tensor(out=PTbd[hs, cg, hs], in0=pq[hs, 128 + hh * 64:192 + hh * 64], scalar=em[hs, cg:cg + 1],
                                                                 in1=DG[hs, cg, :], op0=ALU.mult, op1=ALU.add),
                         reads=[tpq, tsm, tDG], writes=[tPT[cg]])
                P.op("pe", lambda e: e.matmul(pq[:, 256:512].rearrange("p (a b) -> p a b", a=2), BpT[:, csl], Z[:, h0:h0 + 2, 1, :], start=True, stop=False),
                     reads=[tZ[h0], tZ[h1], tBpT], writes=[tpq], same_eng_raw=False)
                P.op("pe", lambda e: e.matmul(pq[:, 256:512].rearrange("p (a b) -> p a b", a=2), KpT[:, csl], Vz[:, h0:h0 + 2, :], start=False, stop=True),
                     reads=[tKpT, tVz], writes=[tpq], same_eng_raw=False)
                P.op("act", lambda e: e.copy(out=Qall[0:64, cg, :], in_=pq[0:64, 256:320]), reads=[tpq], writes=[tQ])
                P.op("act", lambda e: e.copy(out=Qall[64:128, cg, :], in_=pq[64:128, 448:512]), reads=[tpq], writes=[tQ])
                for hh in range(2):
                    h = 2 * cg + hh
                    hs = slice(hh * 64, hh * 64 + 64)
                    yv = pc[:, h // 8, (h % 8) * 64:(h % 8) * 64 + 64]
                    P.op("pe", lambda e: e.matmul(yv, LR[hh][:, 0:128], Z[:, h, 1, hs], start=True, stop=False),
                         reads=[tLR[hh], tZ[h]], writes=[tpc[h // 8]], same_eng_raw=False)
                    P.op("pe", lambda e: e.matmul(yv, LR[hh][:, 256:384], Vz[:, h, hs], start=False, stop=False),
                         reads=[tLR[hh], tVz], writes=[tpc[h // 8]], same_eng_raw=False)
                    P.op("pe", lambda e: e.matmul(yv, RhT[hs, cg, :], Sp[hs, cg, :], start=False, stop=True),
                         reads=[tRh[cg], tSp], writes=[tpc[h // 8]], same_eng_raw=False)
                P.op("pe", lambda e: e.matmul(pS[:, cg, :], PTbd[:, cg, :], Sp[:, cg, :], start=True, stop=True),
                     reads=[tPT[cg], tSp], writes=[tpS], same_eng_raw=False)
            P.op("dve", lambda e: e.tensor_tensor(out=Sn[:], in0=pS[:], in1=Qall[:], op=ALU.add), reads=[tpS, tQ], writes=[tSn])
            for a in range(2):
                P.op("act", lambda e: e.copy(out=ysb[:, a * 8:(a + 1) * 8, :], in_=pc[:, a, :].rearrange("p (h v) -> p h v", v=64)),
                     reads=[tpc[a]], writes=[tysb])
            P.op("dve", lambda e: e.tensor_reduce(out=st1[:, 0, :], in_=ysb[:], axis=AX.X, op=ALU.add), reads=[tysb], writes=[tst])
            P.op("pool", lambda e: e.tensor_tensor(out=ysq[:], in0=ysb[:], in1=ysb[:], op=ALU.mult), reads=[tysb], writes=[tysq])
            P.op("dve", lambda e: e.tensor_reduce(out=st1[:, 1, :], in_=ysq[:], axis=AX.X, op=ALU.add), reads=[tysq], writes=[tst])
            P.op("dve", lambda e: e.tensor_scalar(out=st1[:, 0, :], in0=st1[:, 0, :], scalar1=1.0 / 64, scalar2=None, op0=ALU.mult), reads=[tst], writes=[tst])
            P.op("dve", lambda e: e.tensor_tensor(out=st1[:, 2, :], in0=st1[:, 0, :], in1=st1[:, 0, :], op=ALU.mult), reads=[tst], writes=[tst])
            P.op("dve", lambda e: e.scalar_tensor_tensor(out=st1[:, 1, :], in0=st1[:, 1, :], scalar=1.0 / 64, in1=st1[:, 2, :], op0=ALU.mult, op1=ALU.subtract),
                 reads=[tst], writes=[tst])
            P.op("dve", lambda e: e.tensor_scalar(out=st1[:, 3, :], in0=st1[:, 1, :], scalar1=GN_EPS, scalar2=None, op0=ALU.add), reads=[tst], writes=[tst])
            P.op("act", lambda e: e.activation(out=st1[:, 3, :], in_=st1[:, 3, :], func=AF.Sqrt), reads=[tst], writes=[tst])
            P.op("dve", lambda e: e.reciprocal(out=st1[:, 3, :], in_=st1[:, 3, :]), reads=[tst], writes=[tst])

            def bc16(ap2):
                return ap2.unsqueeze(2).to_broadcast([128, 16, 64])
            P.op("dve", lambda e: e.tensor_tensor(out=ysb[:], in0=ysb[:], in1=bc16(st1[:, 0, :]), op=ALU.subtract), reads=[tysb, tst], writes=[tysb])
            P.op("pool", lambda e: e.tensor_tensor(out=ysb[:], in0=ysb[:], in1=bc16(st1[:, 3, :]), op=ALU.mult), reads=[tysb, tst], writes=[tysb])
            ysf = ysb[:].rearrange("p h v -> p (h v)")
            P.op("dve", lambda e: e.tensor_tensor(out=ysf, in0=ysf, in1=lnS[:, 0, :], op=ALU.mult), reads=[tysb, tC], writes=[tysb])
            P.op("pool", lambda e: e.tensor_tensor(out=ysf, in0=ysf, in1=lnS[:, 1, :], op=ALU.add), reads=[tysb, tC], writes=[tysb])
            P.op("pool", lambda e: e.tensor_tensor(out=ysq[:], in0=V_.rearrange("p (h v) -> p h v", v=64), in1=bc16(bon[:]), op=ALU.mult),
                 reads=[ttm[b], tbon], writes=[tysq])
            P.op("dve", lambda e: e.tensor_tensor(out=ysb[:], in0=ysb[:], in1=ysq[:], op=ALU.add), reads=[tysb, tysq], writes=[tysb])
            P.op("pool", lambda e: e.tensor_tensor(out=yout[:], in0=ysf, in1=GS_, op=ALU.mult), reads=[tysb, ttm[b]], writes=[tyo])
            P.dma("sp", yo[n], yout[:], reads=[tyo])
        P.wait_all("sp", [tyo])
        print("scan insts", P.n_inst)
    return nc


NORM_EPS = 1e-6
TOK = 2048


class Stage:
    def __init__(self, name):
        self.nc = bass.Bass("TRN2", target_bir_lowering=False)
        self.st = contextlib.ExitStack()
        self.P = Prog(self.nc, self.st)
        P = self.P
        self.AT = None; self.tAT = Trk("AT")
        self.wring = [P.sb(f"wt{i}", [128, 32, 128], BF16) for i in range(3)]; self.twr = [Trk(f"wt{i}") for i in range(3)]
        self.wi = 0
        self.pg = [P.ps(f"pg{i}", [128, 512]) for i in range(4)]; self.tpg = [Trk(f"pg{i}") for i in range(4)]
        self.pgi = 0
        self.ssum = P.ps("ssum", [128, 4, 512]); self.tss = [Trk(f"ss{i}") for i in range(4)]
        self.stg = [P.sb(f"stg{i}", [128, 512], F32) for i in range(4)]; self.tstg = [Trk(f"stg{i}") for i in range(4)]
        self.stb = [P.sb(f"stb{i}", [128, 512], BF16) for i in range(4)]; self.tstb = [Trk(f"stb{i}") for i in range(4)]
        self.si = 0
        self.ones_b = P.sb("ones_b", [128, 128], BF16); self.tC = Trk("C")
        P.op("pool", lambda e: e.memset(self.ones_b[:], 1.0), writes=[self.tC])
        self.rstd = P.sb("rstd", [128, TOK], F32); self.trstd = Trk("rstd")

    def alloc_AT(self, fence=()):
        self.AT = self.P.sb("AT", [128, 32, TOK], BF16)
        self.P.wait_all("sp", list(fence))

    def tmp_stack(self):
        st2 = contextlib.ExitStack()
        return st2

    def din(self, name, shape, dt=F32):
        return self.nc.dram_tensor(name, list(shape), dt, kind="ExternalInput").ap()

    def dout(self, name, shape, dt=F32):
        return self.nc.dram_tensor(name, list(shape), dt, kind="ExternalOutput").ap()

    def dscr(self, name, shape, dt=F32):
        return self.nc.dram_tensor(name, list(shape), dt, kind="Internal").ap()

    def stage(self, dt=F32):
        i = self.si % 4
        self.si += 1
        return (self.stg[i], self.tstg[i]) if dt == F32 else (self.stb[i], self.tstb[i])

    def gemm(self, W, n0, N, KC, epi, AT=None, tAT=None):
        P = self.P
        AT = self.AT if AT is None else AT
        tAT = self.tAT if tAT is None else tAT
        for j in range(N // 128):
            wt, twt = self.wring[self.wi % 3], self.twr[self.wi % 3]
            self.wi += 1
            src = W[:, n0 + j * 128:n0 + (j + 1) * 128]
            if KC > 1:
                src = src.rearrange("(c p) n -> p c n", p=128)
                P.dma("pool", wt[:, 0:KC, :], src, writes=[twt])
            else:
                P.dma("pool", wt[:, 0, :], src, writes=[twt])
            for tb in range(TOK // 512):
                pb, tpb = self.pg[self.pgi % 4], self.tpg[self.pgi % 4]
                self.pgi += 1
                for c in range(KC):
                    P.op("pe", lambda e: e.matmul(pb[:], wt[:, c, :], AT[:, c, tb * 512:(tb + 1) * 512], start=(c == 0), stop=(c == KC - 1)),
                         reads=[twt, tAT], writes=[tpb], same_eng_raw=False)
                epi(j, tb, pb, tpb)

    def sumsq_acc(self, src_ap, reads, tb, first, last):
        P = self.P
        sq, tsq = self.stage(BF16)
        P.op("act", lambda e: e.activation(out=sq[:], in_=src_ap, func=AF.Square), reads=reads, writes=[tsq])
        P.op("pe", lambda e: e.matmul(self.ssum[:, tb, :], self.ones_b[:], sq[:], start=first, stop=last),
             reads=[tsq, self.tC], writes=[self.tss[tb]], same_eng_raw=False)

    def make_rstd(self):
        P = self.P
        for tb in range(4):
            sl = slice(tb * 512, (tb + 1) * 512)
            P.op("dve", lambda e: e.tensor_scalar(out=self.rstd[:, sl], in0=self.ssum[:, tb, :], scalar1=1.0 / 4096, scalar2=NORM_EPS,
                                                  op0=ALU.mult, op1=ALU.add), reads=[self.tss[tb]], writes=[self.trstd])
        P.op("act", lambda e: e.activation(out=self.rstd[:], in_=self.rstd[:], func=AF.Sqrt), reads=[self.trstd], writes=[self.trstd])
        P.op("dve", lambda e: e.reciprocal(out=self.rstd[:], in_=self.rstd[:]), reads=[self.trstd], writes=[self.trstd])

    def finish(self, trks):
        self.P.wait_everything("sp")
        self.st.close()
        return self.nc


def build_A1():
    S = Stage("A1"); P = S.P
    xT = S.din("xT", [4096, TOK]); xp = S.din("xp", [128, 32])
    vec = S.din("vec", [128, 9, 32])
    w_in = S.din("w_in", [4, 4096, 4096]); w1 = S.din("w1", [4096, 128]); w2 = S.din("w2", [128, 4096])
    a1 = S.din("a1", [4096, 128]); a2 = S.din("a2", [128, 4096])
    outs = [S.dout(n, [4096, TOK]) for n in ("rT", "kT", "vT", "gsT", "sgT", "aT")]
    lerp = [S.dscr(f"lerp{i}", [4096, TOK], BF16) for i in range(6)]
    vS = P.sb("vS", [128, 9, 32], F32); tv = Trk("vec")
    P.dma("sp", vS[:], vec, writes=[tv])
    xpS = P.sb("xpS", [128, 32], F32); hnp = P.sb("hnp", [128, 32], F32); tmp32 = P.sb("tmp32", [128, 32], BF16); txp = Trk("xp")
    P.dma("sp", xpS[:], xp, writes=[txp])
    P.op("act", lambda e: e.activation(out=tmp32[:], in_=xpS[:], func=AF.Square), reads=[txp], writes=[txp])
    P.op("pe", lambda e: e.matmul(S.pg[0][:, 0:32], S.ones_b[:], tmp32[:], start=True, stop=True), reads=[txp, S.tC], writes=[S.tpg[0]], same_eng_raw=False)
    rp = P.sb("rp", [128, 1], F32)
    P.op("dve", lambda e: e.tensor_reduce(out=rp[:], in_=S.pg[0][:, 0:32], axis=AX.X, op=ALU.add), reads=[S.tpg[0]], writes=[txp])
    P.op("dve", lambda e: e.tensor_scalar(out=rp[:], in0=rp[:], scalar1=1.0 / 4096, scalar2=NORM_EPS, op0=ALU.mult, op1=ALU.add), reads=[txp], writes=[txp])
    P.op("act", lambda e: e.activation(out=rp[:], in_=rp[:], func=AF.Sqrt), reads=[txp], writes=[txp])
    P.op("dve", lambda e: e.reciprocal(out=rp[:], in_=rp[:]), reads=[txp], writes=[txp])
    P.op("dve", lambda e: e.scalar_tensor_tensor(out=hnp[:], in0=xpS[:], scalar=rp[:, 0:1], in1=vS[:, 0, :], op0=ALU.mult, op1=ALU.mult), reads=[txp, tv], writes=[txp])
    st2 = contextlib.ExitStack()
    sb2 = lambda name, shape, dt: st2.enter_context(S.nc.sbuf_tensor(name, list(shape), dt))
    xc = [sb2(f"xc{i}", [128, TOK], F32) for i in range(2)]; txc = [Trk("xc0"), Trk("xc1")]
    for c in range(32):
        b = c % 2
        P.dma("sp", xc[b][:], xT[c * 128:(c + 1) * 128, :], writes=[txc[b]])
        for tb in range(4):
            S.sumsq_acc(xc[b][:, tb * 512:(tb + 1) * 512], [txc[b]], tb, c == 0, c == 31)
    S.make_rstd()
    hn = sb2("hn", [128, TOK], F32); thn = Trk("hn")
    dx = sb2("dx", [128, TOK], F32); tdx = Trk("dx")
    lo = [sb2(f"lo{i}", [128, TOK], BF16) for i in range(3)]; tlo = [Trk(f"lo{i}") for i in range(3)]
    li = 0
    for c in range(32):
        b = c % 2
        P.dma("sp", xc[b][:], xT[c * 128:(c + 1) * 128, :], writes=[txc[b]])
        P.op("dve", lambda e: e.scalar_tensor_tensor(out=hn[:], in0=xc[b][:], scalar=vS[:, 0, c:c + 1], in1=S.rstd[:], op0=ALU.mult, op1=ALU.mult),
             reads=[txc[b], tv, S.trstd], writes=[thn])
        P.op("pool", lambda e: e.tensor_tensor(out=dx[:, 1:TOK], in0=hn[:, 0:TOK - 1], in1=hn[:, 1:TOK], op=ALU.subtract), reads=[thn], writes=[tdx])
        P.op("pool", lambda e: e.tensor_tensor(out=dx[:, 0:1], in0=hnp[:, c:c + 1], in1=hn[:, 0:1], op=ALU.subtract), reads=[thn, txp], writes=[tdx])
        for i in range(6):
            l_, tl_ = lo[li % 3], tlo[li % 3]; li += 1
            eng = "dve"
            P.op(eng, lambda e: e.scalar_tensor_tensor(out=l_[:], in0=dx[:], scalar=vS[:, 1 + i, c:c + 1], in1=hn[:], op0=ALU.mult, op1=ALU.add),
                 reads=[tdx, thn, tv], writes=[tl_])
            P.dma("act", lerp[i][c * 128:(c + 1) * 128, :], l_[:], reads=[tl_])
    tlerp_done = tlo
    st2.close()
    S.alloc_AT(fence=txc + [thn, tdx] + tlo)
    hw = P.sb("hw", [128, 1, TOK], BF16); thw = Trk("hw")

    def load_AT(i):
        for q in range(4):
            P.dma("sp", S.AT[:, q * 8:(q + 1) * 8, :], lerp[i][q * 1024:(q + 1) * 1024, :].rearrange("(c p) t -> p c t", p=128),
                  reads=tlerp_done, writes=[S.tAT])

    def epi_out(out_ap, func):
        def f(j, tb, pb, tpb):
            s_, ts_ = S.stage(F32)
            P.op("act", lambda e: e.activation(out=s_[:], in_=pb[:], func=func), reads=[tpb], writes=[ts_])
            P.dma("sp", out_ap[j * 128:(j + 1) * 128, tb * 512:(tb + 1) * 512], s_[:], reads=[ts_])
        return f

    def epi_hw(func):
        def f(j, tb, pb, tpb):
            P.op("act", lambda e: e.activation(out=hw[:, 0, tb * 512:(tb + 1) * 512], in_=pb[:], func=func), reads=[tpb], writes=[thw])
        return f

    def epi_sig(out_ap, kind):
        def f(j, tb, pb, tpb):
            s_, ts_ = S.stage(F32)
            P.op("act", lambda e: e.activation(out=s_[:], in_=pb[:], func=AF.Sigmoid, bias=vS[:, kind, j:j + 1], scale=1.0), reads=[tpb, tv], writes=[ts_])
            P.dma("sp", out_ap[j * 128:(j + 1) * 128, tb * 512:(tb + 1) * 512], s_[:], reads=[ts_])
        return f

    for i in range(4):
        load_AT(i)
        S.gemm(w_in[i], 0, 4096, 32, epi_out(outs[i], AF.Silu if i == 3 else AF.Copy))
    load_AT(4)
    S.gemm(w1, 0, 128, 32, epi_hw(AF.Tanh))
    S.gemm(w2, 0, 4096, 1, epi_sig(outs[4], 7), AT=hw, tAT=thw)
    load_AT(5)
    S.gemm(a1, 0, 128, 32, epi_hw(AF.Copy))
    S.gemm(a2, 0, 4096, 1, epi_sig(outs[5], 8), AT=hw, tAT=thw)
    print("A1 insts", P.n_inst)
    return S.finish(S.tstg)


def wo_norm_res(S, AsrcT, resT, w_o, gcol, outT, tout, mix, cast_load=True, stop=99, out2=None):
    P = S.P
    for q in range(4):
        P.dma("pool", S.AT[:, q * 8:(q + 1) * 8, :], AsrcT[q * 1024:(q + 1) * 1024, :].rearrange("(c p) t -> p c t", p=128), writes=[S.tAT])
    tmix = [Trk(f"mix{j}") for j in range(32)]

    def epi(j, tb, pb, tpb):
        s_, ts_ = S.stage(F32)
        P.op("act", lambda e: e.copy(out=s_[:], in_=pb[:]), reads=[tpb], writes=[ts_])
        P.dma("sp", mix[j * 128:(j + 1) * 128, tb * 512:(tb + 1) * 512], s_[:], reads=[ts_], writes=[tmix[j]])
        S.sumsq_acc(pb[:], [tpb], tb, j == 0, j == 31)
    if stop <= 0:
        return
    S.gemm(w_o, 0, 4096, 32, epi)
    S.make_rstd()
    if stop <= 1:
        return
    for j in range(32):
        b = j % 2
        P.dma("sp", S.mc[b][:], mix[j * 128:(j + 1) * 128, :], reads=[tmix[j]], writes=[S.tmc[b]])
        P.dma("act", S.xc[b][:], resT[j * 128:(j + 1) * 128, :], writes=[S.txc[b]])
        P.op("dve", lambda e: e.scalar_tensor_tensor(out=S.mc[b][:], in0=S.mc[b][:], scalar=gcol(j), in1=S.rstd[:], op0=ALU.mult, op1=ALU.mult),
             reads=[S.tmc[b], S.trstd, S.tv], writes=[S.tmc[b]])
        P.op("pool", lambda e: e.tensor_tensor(out=S.mc[b][:], in0=S.mc[b][:], in1=S.xc[b][:], op=ALU.add), reads=[S.tmc[b], S.txc[b]], writes=[S.tmc[b]])
        P.dma("sp", outT[j * 128:(j + 1) * 128, :], S.mc[b][:], reads=[S.tmc[b]], writes=[tout[j]])
        if out2 is not None:
            P.dma("act", out2[j * 128:(j + 1) * 128, :], S.mc[b][:], reads=[S.tmc[b]], writes=[tout[j]])
        yield j, S.mc[b], S.tmc[b]


def alloc_chunks(S):
    P = S.P
    S.mc = [P.sb(f"mc{i}", [128, TOK], F32) for i in range(2)]; S.tmc = [Trk("mc0"), Trk("mc1")]
    S.xc = [P.sb(f"xcb{i}", [128, TOK], F32) for i in range(2)]; S.txc = [Trk("xcb0"), Trk("xcb1")]


def build_B(stop=99):
    S = Stage("B"); P = S.P
    yT = S.din("yT", [4096, TOK]); xT = S.din("xT", [4096, TOK]); vec = S.din("vec", [128, 3, 32])
    w_o = S.din("w_o", [4096, 4096]); w_k = S.din("w_k", [4096, 4096]); w_v = S.din("w_v", [4096, 4096]); w_qg = S.din("w_qg", [4096, 8192])
    h1T = S.dout("h1T", [4096, TOK]); kT = S.dout("kT", [4096, TOK]); vT = S.dout("vT", [4096, TOK])
    qT = S.dout("qT", [4096, TOK]); gsT = S.dout("gsT", [4096, TOK]); kmT = S.dout("kmT", [4096, 8])
    mix = S.dscr("mix", [4096, TOK]); h1s = S.dscr("h1s", [4096, TOK])
    S.vS = P.sb("vS", [128, 3, 32], F32); S.tv = Trk("vec")
    P.dma("sp", S.vS[:], vec, writes=[S.tv])
    alloc_chunks(S)
    S.alloc_AT()
    th1 = [Trk(f"h1_{j}") for j in range(32)]
    for j, h1c, th in wo_norm_res(S, yT, xT, w_o, lambda j: S.vS[:, 0, j:j + 1], h1T, th1, mix, stop=stop, out2=h1s):
        for tb in range(4):
            S.sumsq_acc(h1c[:, tb * 512:(tb + 1) * 512], [th], tb, j == 0, j == 31)
    if stop <= 2:
        return S.finish([])
    S.make_rstd()
    kms = P.sb("kms", [128, 32, 8], F32); tkm = Trk("kms")
    douts = []

    def load_norm(kind):
        for c in range(32):
            b = c % 2
            P.dma("sp", S.mc[b][:], h1s[c * 128:(c + 1) * 128, :], reads=[th1[c]], writes=[S.tmc[b]])
            P.op("dve", lambda e: e.scalar_tensor_tensor(out=S.AT[:, c, :], in0=S.mc[b][:], scalar=S.vS[:, kind, c:c + 1], in1=S.rstd[:],
                                                         op0=ALU.mult, op1=ALU.mult), reads=[S.tmc[b], S.trstd, S.tv], writes=[S.tAT])

    def epi_out(out_ap, dt, func, km=False):
        def f(j, tb, pb, tpb):
            s_, ts_ = S.stage(dt)
            P.op("act", lambda e: e.activation(out=s_[:], in_=pb[:], func=func), reads=[tpb], writes=[ts_])
            douts.append(ts_)
            P.dma("sp", out_ap[j * 128:(j + 1) * 128, tb * 512:(tb + 1) * 512], s_[:], reads=[ts_])
            if km:
                P.op("dve", lambda e: e.tensor_reduce(out=kms[:, j, tb * 2:(tb + 1) * 2], in_=s_[:].rearrange("p (a b) -> p a b", a=2), axis=AX.X, op=ALU.add),
                     reads=[ts_], writes=[tkm])
        return f
    load_norm(1)
    if stop <= 3:
        return S.finish([])
    S.gemm(w_k, 0, 4096, 32, epi_out(kT, F32, AF.Copy, km=True))
    if stop <= 4:
        return S.finish([])
    S.gemm(w_v, 0, 4096, 32, epi_out(vT, F32, AF.Copy))
    P.op("dve", lambda e: e.tensor_scalar(out=kms[:], in0=kms[:], scalar1=1.0 / 256, scalar2=None, op0=ALU.mult), reads=[tkm], writes=[tkm])
    P.dma("sp", kmT.rearrange("(j p) b -> p j b", p=128), kms[:], reads=[tkm])
    load_norm(2)
    S.gemm(w_qg, 0, 4096, 32, epi_out(qT, F32, AF.Copy))
    S.gemm(w_qg, 4096, 4096, 32, epi_out(gsT, F32, AF.Silu))
    print("B insts", P.n_inst)
    return S.finish(S.tstg + S.tstb + [tkm] + S.tmc)


def build_D():
    S = Stage("D"); P = S.P
    aT = S.din("aT", [4096, TOK]); hT = S.din("hT", [4096, TOK]); vec = S.din("vec", [128, 1, 32])
    w_o = S.din("w_o", [4096, 4096])
    oT = S.dout("oT", [4096, TOK])
    mix = S.dscr("mix", [4096, TOK])
    S.vS = P.sb("vS", [128, 1, 32], F32); S.tv = Trk("vec")
    P.dma("sp", S.vS[:], vec, writes=[S.tv])
    alloc_chunks(S)
    S.alloc_AT()
    to = [Trk(f"o_{j}") for j in range(32)]
    for _ in wo_norm_res(S, aT, hT, w_o, lambda j: S.vS[:, 0, j:j + 1], oT, to, mix):
        pass
    print("D insts", P.n_inst)
    return S.finish(S.tmc + to)


SCALE = 128 ** -0.5
NEG = -1.0e30


def build_C(NH, T=8192, stop=99):
    NB = T // 256
    nc = bass.Bass("TRN2", target_bir_lowering=False)
    qT = nc.dram_tensor("qT", [NH, 128, T], F32, kind="ExternalInput").ap()
    kT = nc.dram_tensor("kT", [NH, 128, T], F32, kind="ExternalInput").ap()
    vv = nc.dram_tensor("v", [NH, T, 128], F32, kind="ExternalInput").ap()
    km = nc.dram_tensor("km", [NH, 128, NB], F32, kind="ExternalInput").ap()
    gs = nc.dram_tensor("gs", [NH, T, 128], F32, kind="ExternalInput").ap()
    hx = nc.dram_tensor("hx", [1, NH], F32, kind="ExternalInput").ap()
    att = nc.dram_tensor("att", [NH, T, 128], F32, kind="ExternalOutput").ap()
    dbg = nc.dram_tensor("dbg", [NH, 128, (T // 256) * 4], F32, kind="ExternalOutput").ap() if stop < 99 else None
    with contextlib.ExitStack() as st:
        P = Prog(nc, st)
        sb, ps = P.sb, P.ps
        tC = Trk("C")
        ones_b = sb("ones_b", [128, 256], BF16)
        maskc = sb("maskc", [128, 2, 256], BF16)
        P.op("pool", lambda e: e.memset(ones_b[:], 1.0), writes=[tC])
        for half in range(2):
            P.op("pool", lambda e: e.affine_select(out=maskc[:, half, :], in_=ones_b[:], pattern=[[1, 256]], compare_op=ALU.is_ge, fill=0.0,
                                                   base=-128 * half, channel_multiplier=-1), reads=[tC], writes=[tC])
        slp = sb("slp", [128, NH], F32); tslp = Trk("slp")
        P.dma("sp", slp[:], hx.partition_broadcast(128), writes=[tslp])
        P.op("act", lambda e: e.activation(out=slp[:], in_=slp[:], func=AF.Exp, scale=-math.log(2.0) / 4.0), reads=[tslp], writes=[tslp])
        Qf = sb("Qf", [128, T], F32); tQf = Trk("Qf")
        Qb = sb("Qb", [128, T], BF16); tQb = Trk("Qb")
        Kb = sb("Kb", [128, T], BF16); tKb = Trk("Kb")
        V1 = sb("V1", [128, T // 128, 130], BF16); tV1 = Trk("V1")
        P.op("pool", lambda e: e.memset(V1[:, :, 128:129], 1.0), writes=[tV1])
        kmS = sb("kmS", [128, NB], F32); tkm = Trk("km")
        G = sb("G", [128, T // 128, 128], F32); tG = Trk("G")
        bias_i = sb("bias_i", [128, NB * 2 * 4], mybir.dt.int32)
        biasq = [sb(f"biasq{i}", [128, NB, 2], F32) for i in range(2)]; tbias = Trk("bias")
        gt = [sb(f"gt{i}", [128, 32], F32) for i in range(2)]; tgt = [Trk("gt0"), Trk("gt1")]
        m8 = [sb(f"m8{i}", [128, 8], F32) for i in range(2)]
        sel = [sb(f"sel{i}", [128, 32], F32) for i in range(2)]; tsel = [Trk("sel0"), Trk("sel1")]
        acc = [sb(f"acc{i}", [128, 129], F32) for i in range(2)]; tacc = [Trk("acc0"), Trk("acc1")]
        rec = [sb(f"rec{i}", [128, 1], F32) for i in range(2)]
        ot = [sb(f"ot{i}", [128, 128], F32) for i in range(2)]; tot = [Trk("ot0"), Trk("ot1")]
        PT = [[[sb(f"PT{r}_{h}_{q}", [128, 128], BF16) for q in range(2)] for h in range(2)] for r in range(3)]
        tPT = [[[Trk(f"PT{r}_{h}_{q}") for q in range(2)] for h in range(2)] for r in range(3)]
        sc = [ps(f"sc{i}", [128, 512]) for i in range(3)]; tsc = [[Trk(f"sc{i}_{k}") for k in range(4)] for i in range(3)]
        po = [ps(f"po{i}", [128, 512]) for i in range(3)]; tpob = [Trk(f"pob{i}") for i in range(3)]
        tscb = [Trk(f"scb{i}") for i in range(3)]
        pgt = ps("pgt", [128, 512]); tpgt = Trk("pgt")
        sci = 0; poi = 0; pti = 0
        for hi in range(NH):
            for q in range(4):
                tsl = slice(q * (T // 4), (q + 1) * (T // 4))
                P.dma("sp", Qf[:, tsl], qT[hi][:, tsl], writes=[tQf])
                P.dma("pool", Kb[:, tsl], kT[hi][:, tsl], writes=[tKb])
                csl = slice(q * (T // 512), (q + 1) * (T // 512))
                P.dma("pool", V1[:, csl, 0:128], vv[hi][tsl, :].rearrange("(c p) d -> p c d", p=128), writes=[tV1])
                P.dma("act", G[:, csl, :], gs[hi][tsl, :].rearrange("(c p) d -> p c d", p=128), writes=[tG])
                P.op("pool", lambda e: e.tensor_copy(out=Qb[:, tsl], in_=Qf[:, tsl]), reads=[tQf], writes=[tQb])
            P.dma("sp", kmS[:], km[hi], writes=[tkm])
            CST = 30.0
            for qs in range(2):
                imax = qs * 128 + 127
                bq = biasq[qs][:].rearrange("p d h -> p (d h)")
                P.op("pool", lambda e: e.iota(bias_i[:, 0:NB * 2], pattern=[[-256, NB], [128, 2], [-256, 1]], base=-imax, channel_multiplier=1),
                     writes=[tbias])
                P.op("pool", lambda e: e.tensor_copy(out=bq, in_=bias_i[:, 0:NB * 2]), reads=[tbias], writes=[tbias])
                P.op("dve", lambda e: e.tensor_scalar(out=bq, in0=bq, scalar1=slp[:, hi:hi + 1], scalar2=None, op0=ALU.mult), reads=[tbias, tslp], writes=[tbias])
                P.op("dve", lambda e: e.tensor_scalar(out=bq, in0=bq, scalar1=CST, scalar2=None, op0=ALU.add), reads=[tbias], writes=[tbias])
            if stop < 99:
                for qs in range(2):
                    P.dma("sp", dbg[hi][:, qs * NB * 2:(qs + 1) * NB * 2], biasq[qs][:].rearrange("p d h -> p (d h)"), reads=[tbias])
            if stop <= 1:
                continue
            for m in range(NB):
                q0 = m * 256
                if m > 0:
                    for qs in range(2):
                        P.op("pe", lambda e: e.matmul(pgt[:, qs * 32:qs * 32 + NB], Qf[:, q0 + qs * 128:q0 + qs * 128 + 128], kmS[:], start=True, stop=True),
                             reads=[tQf, tkm], writes=[tpgt], same_eng_raw=False)
                        P.op("pool", lambda e: e.memset(gt[qs][:], NEG), writes=[tgt[qs]])
                        P.op("dve", lambda e: e.tensor_copy(out=gt[qs][:, 0:m], in_=pgt[:, qs * 32:qs * 32 + m]), reads=[tpgt], writes=[tgt[qs]])
                        P.op("dve", lambda e: e.max(m8[qs][:], gt[qs][:]), reads=[tgt[qs]], writes=[tsel[qs]])
                        P.op("dve", lambda e: e.tensor_scalar(out=sel[qs][:], in0=gt[qs][:], scalar1=m8[qs][:, 2:3], scalar2=None, op0=ALU.is_ge),
                             reads=[tgt[qs], tsel[qs]], writes=[tsel[qs]])
                if stop <= 2:
                    continue
                for n in range(m, -1, -1):
                    d = m - n
                    own = (d == 0)
                    pt_, tpt_ = PT[pti % 3], tPT[pti % 3]; pti += 1
                    s_, ts_ = sc[sci % 3], tscb[sci % 3]; sci += 1
                    tiles = [(half, qs) for half in range(2) for qs in range(2) if not (own and half == 1 and qs == 0)]
                    for (half, qs) in tiles:
                        k0 = n * 256 + half * 128
                        so = (half * 2 + qs) * 128
                        P.op("pe", lambda e: e.matmul(s_[:, so:so + 128], Kb[:, k0:k0 + 128], Qb[:, q0 + qs * 128:q0 + qs * 128 + 128], start=True, stop=True),
                             reads=[tKb, tQb], writes=[ts_], same_eng_raw=False)
                    for (half, qs) in tiles:
                        so = (half * 2 + qs) * 128
                        P.op("act", lambda e: e.activation(out=pt_[half][qs][:], in_=s_[:, so:so + 128], func=AF.Exp,
                                                           bias=biasq[qs][:, d, half:half + 1], scale=SCALE), reads=[ts_, tbias], writes=[tpt_[half][qs]])
                        if own and half == qs:
                            P.op("dve", lambda e: e.tensor_tensor(out=pt_[half][qs][:], in0=pt_[half][qs][:], in1=maskc[:, 0, 0:128], op=ALU.mult),
                                 reads=[tpt_[half][qs], tC], writes=[tpt_[half][qs]])
                    if stop <= 3:
                        continue
                    o_, to_ = po[poi % 3], tpob[poi % 3]; poi += 1
                    for qs in range(2):
                        oo = qs * 256
                        halves = [0] if (own and qs == 0) else [0, 1]
                        for ih, half in enumerate(halves):
                            P.op("pe", lambda e: e.matmul(o_[:, oo:oo + 129], pt_[half][qs][:], V1[:, n * 2 + half, 0:129],
                                                          start=(ih == 0), stop=(ih == len(halves) - 1)),
                                 reads=[tpt_[half][qs], tV1], writes=[to_], same_eng_raw=False)
                    for qs in range(2):
                        oo = qs * 256
                        if own:
                            P.op("dve", lambda e: e.tensor_copy(out=acc[qs][:], in_=o_[:, oo:oo + 129]), reads=[to_], writes=[tacc[qs]])
                        else:
                            P.op("dve", lambda e: e.scalar_tensor_tensor(out=acc[qs][:], in0=o_[:, oo:oo + 129], scalar=sel[qs][:, n:n + 1], in1=acc[qs][:],
                                                                         op0=ALU.mult, op1=ALU.add), reads=[to_, tsel[qs], tacc[qs]], writes=[tacc[qs]])
                if stop <= 3:
                    continue
                for qs in range(2):
                    P.op("dve", lambda e: e.reciprocal(out=rec[qs][:], in_=acc[qs][:, 128:129]), reads=[tacc[qs]], writes=[tacc[qs]])
                    P.op("dve", lambda e: e.scalar_tensor_tensor(out=ot[qs][:], in0=acc[qs][:, 0:128], scalar=rec[qs][:, 0:1], in1=G[:, m * 2 + qs, :],
                                                                 op0=ALU.mult, op1=ALU.mult), reads=[tacc[qs], tG], writes=[tot[qs]])
                    P.dma("sp", att[hi][q0 + qs * 128:q0 + qs * 128 + 128, :], ot[qs][:], reads=[tot[qs]])
        P.wait_everything("sp")
        print("C insts", P.n_inst)
    return nc


from concourse.bass_utils import run_bass_kernel_spmd
import ml_dtypes as _mld


def _pc(v):
    return np.ascontiguousarray(v.reshape(32, 128).T)


def _run(nc, maps):
    return run_bass_kernel_spmd(nc, maps, core_ids=list(range(8))).results


def _tokT(arr, c):
    b, s = c // 4, (c % 4) * TOK
    return np.ascontiguousarray(arr[b, s:s + TOK].T)


def _gather_T(res, name, dtype=np.float32):
    out = np.empty((2, 8192, 4096), dtype)
    for c in range(8):
        b, s = c // 4, (c % 4) * TOK
        out[b, s:s + TOK] = np.asarray(res[c][name]).T
    return out


def kernel(**inp):
    inp = {k: np.asarray(v) for k, v in inp.items()}
    x = inp["x"]
    f32 = np.float32
    vecA = np.ascontiguousarray(np.stack([_pc(inp["a_pre_g"][0])] + [_pc(inp["a_mu"][0, i]) for i in range(6)]
                                         + [_pc(inp["a_w0"][0]), _pc(inp["a_a0"][0])], axis=1)).astype(f32)
    maps = []
    for c in range(8):
        b, s = c // 4, (c % 4) * TOK
        xp = np.zeros((128, 32), f32) if s == 0 else _pc(x[b, s - 1])
        maps.append({"xT": _tokT(x, c), "xp": xp, "vec": vecA, "w_in": inp["a_w_in"][0], "w1": inp["a_w1"][0], "w2": inp["a_w2"][0],
                     "a1": inp["a_a1"][0], "a2": inp["a_a2"][0]})
    res = _run(build_A1(), maps)
    r = _gather_T(res, "rT"); k = _gather_T(res, "kT"); v = _gather_T(res, "vT")
    gsA = _gather_T(res, "gsT"); sg = _gather_T(res, "sgT"); a = _gather_T(res, "aT")
    del res, maps
    NCH = 64
    maps = []
    for c in range(8):
        b, g = c // 4, c % 4
        cs = slice(g * 1024, (g + 1) * 1024)

        def fmaj(X):
            return X[b][:, cs].reshape(NCH, 128, 8, 128).transpose(0, 3, 2, 1)
        fm = np.ascontiguousarray(np.stack([fmaj(r), fmaj(k), fmaj(a)], axis=2)).astype(f32)
        tm = np.ascontiguousarray(np.stack([v[b][:, cs].reshape(NCH, 128, 1024), gsA[b][:, cs].reshape(NCH, 128, 1024),
                                            sg[b][:, cs].reshape(NCH, 128, 1024)], axis=2)).astype(f32)

        def cvec(z):
            return z[cs].reshape(8, 128).T
        cv = np.ascontiguousarray(np.stack([cvec(inp["a_k_k"][0]), cvec(inp["a_k_a"][0]), cvec(inp["a_r_k"][0].reshape(-1))], axis=1)).astype(f32)
        ln = np.ascontiguousarray(np.stack([inp["a_lnx_w"][0][cs], inp["a_lnx_b"][0][cs]])).astype(f32)
        maps.append({"fm": fm, "tm": tm, "cv": cv, "ln": ln})
    res = _run(build_scan(NCH), maps)
    y = np.empty((2, 8192, 4096), f32)
    for c in range(8):
        b, g = c // 4, c % 4
        y[b][:, g * 1024:(g + 1) * 1024] = np.asarray(res[c]["y"]).reshape(8192, 1024)
    del res, maps, r, k, v, gsA, sg, a
    vecB = np.ascontiguousarray(np.stack([_pc(inp["a_post_g"][0]), _pc(inp["kv_norm_g"]), _pc(inp["b_pre_g"][0])], axis=1)).astype(f32)
    maps = [{"yT": _tokT(y, c), "xT": _tokT(x, c), "vec": vecB, "w_o": inp["a_w_o"][0], "w_k": inp["w_k"], "w_v": inp["w_v"],
             "w_qg": inp["b_w_qg"][0]} for c in range(8)]
    res = _run(build_B(), maps)
    h1 = _gather_T(res, "h1T"); K = _gather_T(res, "kT"); V = _gather_T(res, "vT")
    Q = _gather_T(res, "qT"); GS = _gather_T(res, "gsT")
    KM = np.empty((2, 32, 4096), f32)
    for c in range(8):
        b, s = c // 4, (c % 4) * 8
        KM[b, s:s + 8] = np.asarray(res[c]["kmT"]).T
    del res, maps, y
    maps = []
    hsets = []
    for c in range(8):
        b, g = c // 4, c % 4
        heads = [g + 4 * i for i in range(8)]
        hsets.append(heads)
        hs = [slice(h * 128, (h + 1) * 128) for h in heads]
        maps.append({"qT": np.ascontiguousarray(np.stack([Q[b][:, s_].T for s_ in hs])),
                     "kT": np.ascontiguousarray(np.stack([K[b][:, s_].T for s_ in hs])),
                     "v": np.ascontiguousarray(np.stack([V[b][:, s_] for s_ in hs])),
                     "km": np.ascontiguousarray(np.stack([KM[b][:, s_].T for s_ in hs])),
                     "gs": np.ascontiguousarray(np.stack([GS[b][:, s_] for s_ in hs])),
                     "hx": np.array([[h + 1 for h in heads]], np.float32)})
    att = np.empty((2, 8192, 4096), f32)
    res = _run(build_C(8), maps)
    for c in range(8):
        b = c // 4
        for i, h in enumerate(hsets[c]):
            att[b][:, h * 128:(h + 1) * 128] = np.asarray(res[c]["att"])[i]
    del res, maps, K, V, Q, GS
    vecD = np.ascontiguousarray(_pc(inp["b_post_g"][0])[:, None, :]).astype(f32)
    maps = [{"aT": _tokT(att, c), "hT": _tokT(h1, c), "vec": vecD, "w_o": inp["b_w_o"][0]} for c in range(8)]
    res = _run(build_D(), maps)
    return _gather_T(res, "oT")
```
